# Optimizing a Trainium2 kernel written in Bass

```python
import math
import jax, jax.numpy as jnp
from jax import lax
import numpy as np

D_MODEL = 1024
BATCH = 8
SEQ = 4096
DEPTH = 2

CTX_LEN = 256
GRID_W = 64
N_MOD = 6
D_FF = 4 * D_MODEL
CHUNK = 64
NORM_EPS = 1e-6

HY_C = D_MODEL // 4
HY_ORDER = 2
HY_SHORT = 3
HY_EMB = 33
HY_FH = 64
HY_DECAY_TARGET = 1e-2
HY_FAST_PCT = 0.3
HY_SLOW_PCT = 1.5

GLA_H = 4
GLA_VW = 3 * D_MODEL // 8
GLA_KW = GLA_VW // 2
GLA_DK = GLA_KW // GLA_H
GLA_DV = GLA_VW // GLA_H
GLA_RANK = 16
GLA_TAU = 16.0

GDN_H = 4
GDN_W = 3 * D_MODEL // 8
GDN_D = GDN_W // GDN_H
GDN_SHORT = 7

D_MIX = HY_C + GLA_VW + GDN_W
HY_IN = 3 * HY_C
GLA_IN = 2 * GLA_KW + 2 * GLA_VW + 2 * GLA_RANK
GDN_IN = 4 * GDN_W + 4 * GDN_H
IN_SIZES = (HY_IN, GLA_IN, GDN_IN)
N_IN = HY_IN + GLA_IN + GDN_IN

kernel_name = 'hybrid_hyena_gla_gdn_prefix_dit'

F32 = jnp.float32


def split_last(t, sizes):
    return jnp.split(t, np.cumsum(sizes)[:-1].tolist(), axis=-1)


def rmsnorm(x, g):
    xf = x.astype(F32)
    y = xf * lax.rsqrt(jnp.mean(xf * xf, axis=-1, keepdims=True) + NORM_EPS)
    return (y * g.astype(F32)).astype(x.dtype)


def modulate(x, g, shift, scale):
    return rmsnorm(x, g) * (1 + scale) + shift


def to_heads(t, n_heads):
    b, l, _ = t.shape
    return t.reshape(b, l, n_heads, -1).transpose(0, 2, 1, 3)


def from_heads(t):
    b, h, l, d = t.shape
    return t.transpose(0, 2, 1, 3).reshape(b, l, h * d)


def l2norm(t):
    return t * lax.rsqrt(jnp.sum(t * t, axis=-1, keepdims=True) + NORM_EPS)


def short_conv(u, w, n_seg, seg_len):
    b, l, ch = u.shape
    k = w.shape[0]
    us = u.reshape(b * n_seg, seg_len, ch)
    out = lax.conv_general_dilated(us, w[:, None, :].astype(u.dtype), window_strides=(1,),
                                   padding=[(k // 2, k // 2)], dimension_numbers=('NWC', 'WIO', 'NWC'),
                                   feature_group_count=ch)
    return out.reshape(b, l, ch)


def hyena_filters(length, w1, b1, w2, b2, w3, sin_freq):
    t = jnp.linspace(0.0, 1.0, length, dtype=F32)[:, None]
    bands = (HY_EMB - 1) // 2
    f = jnp.linspace(1e-4, bands - 1, bands, dtype=F32)
    w = 2 * math.pi * jnp.arange(length, dtype=F32) / length
    ang = w[:, None] * f[None, :]
    z = jnp.concatenate([t, jnp.cos(ang), -jnp.sin(ang)], axis=-1)
    h = jnp.sin(sin_freq[0].astype(F32) * (z @ w1.astype(F32) + b1.astype(F32)))
    h = jnp.sin(sin_freq[1].astype(F32) * (h @ w2.astype(F32) + b2.astype(F32)))
    h = (h @ w3.astype(F32)).reshape(length, HY_ORDER, 2, HY_C)
    deltas = jnp.abs(jnp.linspace(math.log(HY_DECAY_TARGET) / HY_SLOW_PCT,
                                  math.log(HY_DECAY_TARGET) / HY_FAST_PCT, HY_C, dtype=F32))
    h = h * jnp.exp(-t * deltas[None, :])[:, None, None, :]
    h_fwd, h_bwd = h[:, :, 0], h[:, :, 1]
    return jnp.concatenate([h_fwd, jnp.zeros_like(h_fwd[:1]), h_bwd[:0:-1]], axis=0)


def hyena_mix(z, n_seg, seg_len, conv_w, conv_b, filt, d_skip):
    u = (short_conv(z, conv_w, n_seg, seg_len) + conv_b).astype(F32)
    v, x1, x2 = jnp.split(u, 3, axis=-1)
    l = u.shape[1]
    filt_f = jnp.fft.rfft(filt, axis=0)
    y = v
    for o, gate in enumerate((x1, x2)):
        conv = jnp.fft.irfft(jnp.fft.rfft(y, n=2 * l, axis=1) * filt_f[None, :, o], n=2 * l, axis=1)[:, :l]
        y = gate * (conv + d_skip[o].astype(F32) * y)
    return y.astype(z.dtype)


def to_chunks(t, n):
    return jnp.moveaxis(t.reshape(t.shape[:2] + (n, CHUNK) + t.shape[3:]), 2, 0)


def from_chunks(t):
    t = jnp.moveaxis(t, 0, 2)
    return t.reshape(t.shape[:2] + (-1,) + t.shape[4:])


def gla_chunk_scan(q, k, v, log_a, s0):
    n = q.shape[2] // CHUNK
    mask = jnp.tril(jnp.ones((CHUNK, CHUNK), bool))[:, :, None]

    def step(s, xs):
        qc, kc, vc, ac = xs
        b = jnp.cumsum(ac, axis=2)
        diff = b[:, :, :, None, :] - b[:, :, None, :, :]
        decay = jnp.exp(jnp.where(mask, diff, -jnp.inf))
        attn = jnp.einsum('bhid,bhjd,bhijd->bhij', qc, kc, decay)
        o = jnp.einsum('bhid,bhde->bhie', qc * jnp.exp(b), s) + jnp.einsum('bhij,bhje->bhie', attn, vc)
        b_last = b[:, :, -1:, :]
        s_new = jnp.exp(b_last[:, :, 0, :])[..., None] * s + jnp.einsum(
            'bhjd,bhje->bhde', kc * jnp.exp(b_last - b), vc)
        return s_new, o

    s_fin, o = lax.scan(step, s0, (to_chunks(q, n), to_chunks(k, n), to_chunks(v, n), to_chunks(log_a, n)))
    return from_chunks(o), s_fin


def gdn_chunk_scan(q, k, v, g, beta, s0):
    n = q.shape[2] // CHUNK
    dk = q.shape[-1]
    strict = jnp.tril(jnp.ones((CHUNK, CHUNK), bool), -1)
    incl = jnp.tril(jnp.ones((CHUNK, CHUNK), bool))
    eye = jnp.eye(CHUNK, dtype=F32)

    def step(s, xs):
        qc, kc, vc, gc, bc = xs
        gcum = jnp.cumsum(gc, axis=-1)
        diff = gcum[..., :, None] - gcum[..., None, :]
        kb = kc * bc[..., None]
        a = -jnp.einsum('bhid,bhjd->bhij', kb, kc) * jnp.exp(jnp.where(strict, diff, -jnp.inf))
        rhs = jnp.concatenate([kb * jnp.exp(gcum)[..., None], vc * bc[..., None]], axis=-1)
        sol = lax.linalg.triangular_solve(eye - a, rhs, left_side=True, lower=True, unit_diagonal=True)
        w, u = sol[..., :dk], sol[..., dk:]
        v_new = u - w @ s
        attn = jnp.einsum('bhid,bhjd->bhij', qc, kc) * jnp.exp(jnp.where(incl, diff, -jnp.inf))
        o = (qc * jnp.exp(gcum)[..., None]) @ s + attn @ v_new
        g_last = gcum[..., -1:]
        s_new = jnp.exp(g_last)[..., None] * s + jnp.einsum(
            'bhjd,bhje->bhde', kc * jnp.exp(g_last - gcum)[..., None], v_new)
        return s_new, o

    s_fin, o = lax.scan(step, s0, (to_chunks(q, n), to_chunks(k, n), to_chunks(v, n),
                                   to_chunks(g, n), to_chunks(beta, n)))
    return from_chunks(o), s_fin


def bidir_scan(scan_fn, shared, fwd, bwd, s0_f, s0_b):
    o_f, s_f = scan_fn(*shared, *fwd, s0_f)
    flip = lambda a: jnp.flip(a, axis=2)
    o_b, s_b = scan_fn(*[flip(a) for a in shared], *[flip(a) for a in bwd], s0_b)
    return o_f + flip(o_b), s_f, s_b


def gated_head_norm(o, gate, g_norm):
    y = o * lax.rsqrt(jnp.mean(o * o, axis=-1, keepdims=True) + NORM_EPS) * g_norm.astype(F32)
    return from_heads(y) * jax.nn.silu(gate.astype(F32))


def gla_prep(z, w_a2, b_a):
    zf = z.astype(F32)
    q, k, v, gate, a_f, a_b = split_last(zf, (GLA_KW, GLA_KW, GLA_VW, GLA_VW, GLA_RANK, GLA_RANK))
    log_a = [to_heads(jax.nn.log_sigmoid(a @ w_a2[d].astype(F32) + b_a[d].astype(F32)) / GLA_TAU, GLA_H)
             for d, a in enumerate((a_f, a_b))]
    shared = (to_heads(q, GLA_H) * GLA_DK ** -0.5, to_heads(k, GLA_H), to_heads(v, GLA_H))
    return shared, (log_a[0],), (log_a[1],), gate


def gla_branch(z_lat, z_ctx, with_ctx_out, w_a2, b_a, norm_g):
    sh_c, fw_c, bw_c, gate_c = gla_prep(z_ctx, w_a2, b_a)
    s0 = jnp.zeros((z_ctx.shape[0], GLA_H, GLA_DK, GLA_DV), F32)
    o_c, s_f, s_b = bidir_scan(gla_chunk_scan, sh_c, fw_c, bw_c, s0, s0)
    sh_l, fw_l, bw_l, gate_l = gla_prep(z_lat, w_a2, b_a)
    o_l, _, _ = bidir_scan(gla_chunk_scan, sh_l, fw_l, bw_l, s_f, s_b)
    out_lat = gated_head_norm(o_l, gate_l, norm_g).astype(z_lat.dtype)
    out_ctx = gated_head_norm(o_c, gate_c, norm_g).astype(z_ctx.dtype) if with_ctx_out else None
    return out_lat, out_ctx


def gdn_prep(z, conv_w, a_log, dt_bias, n_seg, seg_len):
    qkv, gate, a_f, a_b, b_f, b_b = split_last(z, (3 * GDN_W, GDN_W, GDN_H, GDN_H, GDN_H, GDN_H))
    qkv = jax.nn.silu(short_conv(qkv, conv_w, n_seg, seg_len)).astype(F32)
    q, k, v = jnp.split(qkv, 3, axis=-1)
    shared = (l2norm(to_heads(q, GDN_H)) * GDN_D ** -0.5, l2norm(to_heads(k, GDN_H)), to_heads(v, GDN_H))

    def log_decay(a, d):
        return (-jnp.exp(a_log[d].astype(F32)) *
                jax.nn.softplus(a.astype(F32) + dt_bias[d].astype(F32))).transpose(0, 2, 1)

    def write_gate(b):
        return jax.nn.sigmoid(b.astype(F32)).transpose(0, 2, 1)

    return shared, (log_decay(a_f, 0), write_gate(b_f)), (log_decay(a_b, 1), write_gate(b_b)), gate


def gdn_branch(z_lat, z_ctx, rows, with_ctx_out, conv_w, a_log, dt_bias, norm_g):
    sh_c, fw_c, bw_c, gate_c = gdn_prep(z_ctx, conv_w, a_log, dt_bias, 1, z_ctx.shape[1])
    s0 = jnp.zeros((z_ctx.shape[0], GDN_H, GDN_D, GDN_D), F32)
    o_c, s_f, s_b = bidir_scan(gdn_chunk_scan, sh_c, fw_c, bw_c, s0, s0)
    sh_l, fw_l, bw_l, gate_l = gdn_prep(z_lat, conv_w, a_log, dt_bias, rows, GRID_W)
    o_l, _, _ = bidir_scan(gdn_chunk_scan, sh_l, fw_l, bw_l, s_f, s_b)
    out_lat = gated_head_norm(o_l, gate_l, norm_g).astype(z_lat.dtype)
    out_ctx = gated_head_norm(o_c, gate_c, norm_g).astype(z_ctx.dtype) if with_ctx_out else None
    return out_lat, out_ctx


def mixer(h, hc, rows, with_ctx_out, w_in, w_out, hy_conv_w, hy_conv_b, hy_filter, hy_d,
          gla_w_a2, gla_b_a, gla_norm_g, gdn_conv_w, gdn_a_log, gdn_dt_bias, gdn_norm_g):
    zl_hy, zl_gla, zl_gdn = split_last(h @ w_in, IN_SIZES)
    zc_hy, zc_gla, zc_gdn = split_last(hc @ w_in, IN_SIZES)
    hy_lat = hyena_mix(zl_hy, rows, GRID_W, hy_conv_w, hy_conv_b, hyena_filters(h.shape[1], *hy_filter), hy_d)
    gla_lat, gla_ctx = gla_branch(zl_gla, zc_gla, with_ctx_out, gla_w_a2, gla_b_a, gla_norm_g)
    gdn_lat, gdn_ctx = gdn_branch(zl_gdn, zc_gdn, rows, with_ctx_out, gdn_conv_w, gdn_a_log, gdn_dt_bias, gdn_norm_g)
    y_lat = jnp.concatenate([hy_lat, gla_lat, gdn_lat], axis=-1) @ w_out
    if not with_ctx_out:
        return y_lat, None
    hy_ctx = hyena_mix(zc_hy, 1, hc.shape[1], hy_conv_w, hy_conv_b, hyena_filters(hc.shape[1], *hy_filter), hy_d)
    y_ctx = jnp.concatenate([hy_ctx, gla_ctx, gdn_ctx], axis=-1) @ w_out
    return y_lat, y_ctx


def sq_relu_mlp(h, w1, w2):
    return jnp.square(jax.nn.relu(h @ w1)) @ w2


def setup_inputs(seed: int = 0) -> dict:
    key = jax.random.key(seed)
    ks = iter(jax.random.split(key, 32))
    nrm = lambda shape, std: std * jax.random.normal(next(ks), shape, F32)
    dt = jnp.exp(jax.random.uniform(next(ks), (DEPTH, 2, GDN_H), F32, math.log(1e-3), math.log(1e-1)))
    return {
        'x': nrm((BATCH, SEQ, D_MODEL), 1.0),
        'c': nrm((BATCH, D_MODEL), 1.0),
        'ctx': nrm((BATCH, CTX_LEN, D_MODEL), 1.0),
        'c_ctx': nrm((D_MODEL,), 1.0),
        'norm1_g': 1.0 + nrm((DEPTH, D_MODEL), 0.05),
        'norm2_g': 1.0 + nrm((DEPTH, D_MODEL), 0.05),
        'w_mod': nrm((DEPTH, D_MODEL, N_MOD * D_MODEL), D_MODEL ** -0.5),
        'b_mod': nrm((DEPTH, N_MOD * D_MODEL), 0.02),
        'w_in': nrm((DEPTH, D_MODEL, N_IN), D_MODEL ** -0.5),
        'w_out': nrm((DEPTH, D_MIX, D_MODEL), D_MIX ** -0.5),
        'hy_conv_w': nrm((DEPTH, HY_SHORT, HY_IN), HY_SHORT ** -0.5),
        'hy_conv_b': nrm((DEPTH, HY_IN), 0.02),
        'hy_f_w1': nrm((DEPTH, HY_EMB, HY_FH), HY_EMB ** -0.5),
        'hy_f_b1': nrm((DEPTH, HY_FH), 0.02),
        'hy_f_w2': nrm((DEPTH, HY_FH, HY_FH), HY_FH ** -0.5),
        'hy_f_b2': nrm((DEPTH, HY_FH), 0.02),
        'hy_f_w3': nrm((DEPTH, HY_FH, HY_ORDER * 2 * HY_C), 0.05 * HY_FH ** -0.5),
        'hy_sin_freq': 1.0 + nrm((DEPTH, 2, HY_FH), 0.05),
        'hy_d': nrm((DEPTH, HY_ORDER, HY_C), 1.0),
        'gla_w_a2': nrm((DEPTH, 2, GLA_RANK, GLA_KW), GLA_RANK ** -0.5),
        'gla_b_a': nrm((DEPTH, 2, GLA_KW), 0.02),
        'gla_norm_g': 1.0 + nrm((DEPTH, GLA_DV), 0.05),
        'gdn_conv_w': nrm((DEPTH, GDN_SHORT, 3 * GDN_W), GDN_SHORT ** -0.5),
        'gdn_a_log': jnp.log(jax.random.uniform(next(ks), (DEPTH, 2, GDN_H), F32, 1.0, 16.0)),
        'gdn_dt_bias': dt + jnp.log(-jnp.expm1(-dt)),
        'gdn_norm_g': 1.0 + nrm((DEPTH, GDN_D), 0.05),
        'w_mlp1': nrm((DEPTH, D_MODEL, D_FF), D_MODEL ** -0.5),
        'w_mlp2': nrm((DEPTH, D_FF, D_MODEL), D_FF ** -0.5),
        'final_norm_g': 1.0 + nrm((D_MODEL,), 0.05),
    }


def reference(x, c, ctx, c_ctx, norm1_g, norm2_g, w_mod, b_mod, w_in, w_out, hy_conv_w, hy_conv_b,
              hy_f_w1, hy_f_b1, hy_f_w2, hy_f_b2, hy_f_w3, hy_sin_freq, hy_d, gla_w_a2, gla_b_a, gla_norm_g,
              gdn_conv_w, gdn_a_log, gdn_dt_bias, gdn_norm_g, w_mlp1, w_mlp2, final_norm_g):
    rows = x.shape[1] // GRID_W
    silu_c = jax.nn.silu(c)
    silu_cc = jax.nn.silu(c_ctx)
    for l in range(DEPTH):
        with_ctx_out = l < DEPTH - 1
        sh1, sc1, g1, sh2, sc2, g2 = jnp.split((silu_c @ w_mod[l] + b_mod[l])[:, None, :], N_MOD, axis=-1)
        csh1, csc1, cg1, csh2, csc2, cg2 = jnp.split(silu_cc @ w_mod[l] + b_mod[l], N_MOD, axis=-1)
        h = modulate(x, norm1_g[l], sh1, sc1)
        hc = modulate(ctx, norm1_g[l], csh1, csc1)
        hy_filter = (hy_f_w1[l], hy_f_b1[l], hy_f_w2[l], hy_f_b2[l], hy_f_w3[l], hy_sin_freq[l])
        y, yc = mixer(h, hc, rows, with_ctx_out, w_in[l], w_out[l], hy_conv_w[l], hy_conv_b[l], hy_filter,
                      hy_d[l], gla_w_a2[l], gla_b_a[l], gla_norm_g[l], gdn_conv_w[l], gdn_a_log[l],
                      gdn_dt_bias[l], gdn_norm_g[l])
        x = x + g1 * y
        x = x + g2 * sq_relu_mlp(modulate(x, norm2_g[l], sh2, sc2), w_mlp1[l], w_mlp2[l])
        if with_ctx_out:
            ctx = ctx + cg1 * yc
            ctx = ctx + cg2 * sq_relu_mlp(modulate(ctx, norm2_g[l], csh2, csc2), w_mlp1[l], w_mlp2[l])
    return rmsnorm(x, final_norm_g)
```

```python
from contextlib import ExitStack
import math
import numpy as np
import concourse.bass as bass
import concourse.mybir as mybir
from concourse.bass_utils import run_bass_kernel_spmd

F32 = mybir.dt.float32
BF16 = mybir.dt.bfloat16
AF = mybir.ActivationFunctionType
ALU = mybir.AluOpType

D = 1024
SEQ = 4096
CTX = 256
T = SEQ + CTX
NT = T // 128
DEPTH = 2
N_IN = 3504
D_FF = 4096
EPS = 1e-6
HY0 = 0
GLA0 = 768
GDN0 = 768 + 1184

ENGS = ("pe", "act", "dve", "pool", "sp")
N_DMA_SEMS = 24


class Buf:
    __slots__ = ("name", "t", "whole_w", "whole_r", "sub")

    def __init__(self, name, t):
        self.name = name
        self.t = t
        self.whole_w = None
        self.whole_r = []
        self.sub = {}

    def __getitem__(self, idx):
        return self.t[idx]


def _compress(toks):
    d = {}
    for k, v in toks:
        if d.get(k, 0) < v:
            d[k] = v
    return list(d.items())


class Prog:
    def __init__(self, nc):
        self.nc = nc
        self.ops = {e: [] for e in ENGS}
        self.cnt = {e: 0 for e in ENGS}
        self.known = {e: {} for e in ENGS}
        self.snaps = {e: {} for e in ENGS}
        self.dma_cnt = [0] * N_DMA_SEMS
        self.dma_next = 0
        self.dma_last_tok = [None] * N_DMA_SEMS
        self.dma_snaps = {}
        self.n_wait = 0
        self.n_ins = 0

    def _deps(self, reads, writes):
        deps = []
        for key in reads:
            b, k = key if isinstance(key, tuple) else (key, None)
            if b.whole_w is not None:
                deps.append(b.whole_w)
            if k is None:
                for (w, r) in b.sub.values():
                    if w is not None:
                        deps.append(w)
            else:
                s = b.sub.get(k)
                if s is not None and s[0] is not None:
                    deps.append(s[0])
        for key in writes:
            b, k = key if isinstance(key, tuple) else (key, None)
            if b.whole_w is not None:
                deps.append(b.whole_w)
            deps.extend(b.whole_r)
            if k is None:
                for (w, r) in b.sub.values():
                    if w is not None:
                        deps.append(w)
                    deps.extend(r)
            else:
                s = b.sub.get(k)
                if s is not None:
                    if s[0] is not None:
                        deps.append(s[0])
                    deps.extend(s[1])
        return deps

    def _commit(self, tok, reads, writes):
        for key in reads:
            b, k = key if isinstance(key, tuple) else (key, None)
            if k is None:
                b.whole_r.append(tok)
                if len(b.whole_r) > 40:
                    b.whole_r = _compress(b.whole_r)
            else:
                s = b.sub.setdefault(k, [None, []])
                s[1].append(tok)
                if len(s[1]) > 40:
                    s[1] = _compress(s[1])
        for key in writes:
            b, k = key if isinstance(key, tuple) else (key, None)
            if k is None:
                b.whole_w = tok
                b.whole_r = []
                b.sub = {}
            else:
                b.sub[k] = [tok, []]

    def _emit_waits(self, eng, deps):
        kn = self.known[eng]
        need = {}
        for tok in deps:
            if tok is None:
                continue
            semkey, val = tok
            if semkey == eng and eng == "pe":
                continue
            if kn.get(semkey, 0) >= val:
                continue
            if need.get(semkey, 0) < val:
                need[semkey] = val
        for semkey, val in need.items():
            if kn.get(semkey, 0) >= val:
                continue
            self.ops[eng].append(("wait", semkey, val))
            self.n_wait += 1
            kn[semkey] = val
            if isinstance(semkey, str):
                snap = self.snaps[semkey].get(val)
            else:
                snap = self.dma_snaps.get((semkey, val))
            if snap:
                for k2, v2 in snap.items():
                    if kn.get(k2, 0) < v2:
                        kn[k2] = v2

    def op(self, eng, fn, reads=(), writes=(), inc=True):
        deps = self._deps(reads, writes)
        self._emit_waits(eng, deps)
        self.cnt[eng] += 1
        tok = (eng, self.cnt[eng])
        self.ops[eng].append(("ins", fn, self.cnt[eng]))
        self.snaps[eng][self.cnt[eng]] = dict(self.known[eng])
        self.n_ins += 1
        self._commit(tok, reads, writes)
        return tok

    def dma(self, fn, reads=(), writes=(), eng="sp"):
        deps = self._deps(reads, writes)
        s = self.dma_next
        self.dma_next = (self.dma_next + 1) % N_DMA_SEMS
        semkey = ("dma", s)
        if self.dma_last_tok[s] is not None:
            deps.append(self.dma_last_tok[s])
        self._emit_waits(eng, deps)
        self.dma_cnt[s] += 16
        tok = (semkey, self.dma_cnt[s])
        self.dma_last_tok[s] = tok
        self.ops[eng].append(("dma", fn, s))
        self.dma_snaps[(semkey, self.dma_cnt[s])] = dict(self.known[eng])
        self.n_ins += 1
        self._commit(tok, reads, writes)
        return tok

    def emit(self, stack):
        nc = self.nc
        sems = {e: stack.enter_context(nc.semaphore("s_" + e)) for e in ENGS}
        dsems = [stack.enter_context(nc.semaphore("d_%d" % i)) for i in range(N_DMA_SEMS)]
        waited = {e: set() for e in ENGS}
        for e in ENGS:
            for item in self.ops[e]:
                if item[0] == "wait" and isinstance(item[1], str):
                    waited[item[1]].add(item[2])
        rank = {e: {} for e in ENGS}
        for e in ENGS:
            c = 0
            for item in self.ops[e]:
                if item[0] == "ins" and item[2] in waited[e]:
                    c += 1
                    rank[e][item[2]] = c
        self.n_inc = {e: len(rank[e]) for e in ENGS}

        block = stack.enter_context(nc.Block())

        def run(e):
            def body(engine):
                for item in self.ops[e]:
                    if item[0] == "wait":
                        if isinstance(item[1], str):
                            engine.wait_ge(sems[item[1]], rank[item[1]][item[2]])
                        else:
                            engine.wait_ge(dsems[item[1][1]], item[2])
                    elif item[0] == "ins":
                        ins = item[1](engine)
                        if item[2] in rank[e]:
                            ins.then_inc(sems[e], 1)
                    else:
                        ins = item[1](engine)
                        ins.then_inc(dsems[item[2]], 16)
            return body

        block.tensor(run("pe"))
        block.scalar(run("act"))
        block.vector(run("dve"))
        block.gpsimd(run("pool"))
        block.sync(run("sp"))


class Scope(ExitStack):
    def __init__(self, builder):
        super().__init__()
        self.builder = builder
        self.bufs = []

    def __exit__(self, *a):
        rel = self.builder.released
        for b in self.bufs:
            toks = [b.whole_w] + list(b.whole_r)
            for (w, r) in b.sub.values():
                toks.append(w)
                toks.extend(r)
            for t in toks:
                if t is not None and rel.get(t[0], 0) < t[1]:
                    rel[t[0]] = t[1]
        return super().__exit__(*a)


class Builder:
    def __init__(self, nc, debug=False):
        self.nc = nc
        self.P = Prog(nc)
        self.debug = debug
        self.uid = 0
        self.rr = 0
        self.released = {}

    def _new(self, st, name, t):
        b = Buf(name, t)
        b.whole_r = list(self.released.items())
        st.bufs.append(b)
        return b

    def sb(self, st, name, shape, dt):
        self.uid += 1
        return self._new(st, name, st.enter_context(self.nc.sbuf_tensor("%s_%d" % (name, self.uid), shape, dt)))

    def ps(self, st, name, shape, dt):
        self.uid += 1
        return self._new(st, name, st.enter_context(self.nc.psum_tensor("%s_%d" % (name, self.uid), shape, dt)))

    def ring(self, st, name, shape, dt, n, psum=False):
        f = self.ps if psum else self.sb
        return [f(st, "%s%d" % (name, i), shape, dt) for i in range(n)]

    def dram(self, name, shape, dt, kind="Internal"):
        return Buf(name, self.nc.dram_tensor(name, shape, dt, kind=kind).ap())

    def A(self, eng, method, reads, writes, **kw):
        return self.P.op(eng, lambda e: getattr(e, method)(**kw), reads, writes)

    def DMA(self, reads, writes, out, in_, eng="sp"):
        return self.P.dma(lambda e: e.dma_start(out=out, in_=in_), reads, writes, eng=eng)

    def MM(self, reads, writes, out, lhsT, rhs, start, stop):
        return self.P.op("pe", lambda e: e.matmul(out, lhsT=lhsT, rhs=rhs, start=start, stop=stop),
                         reads, writes, inc=stop)

    def TR(self, reads, writes, out, in_, ident, inc=True):
        return self.P.op("pe", lambda e: e.transpose(out=out, in_=in_, identity=ident), reads, writes, inc=inc)

    def evac_eng(self):
        self.rr += 1
        return "act" if self.rr % 2 else "dve"

    def copy(self, eng, src_b, dst_b, out, in_):
        if eng == "act":
            return self.A("act", "activation", [src_b], [dst_b], out=out, in_=in_, func=AF.Copy)
        return self.A(eng, "tensor_copy", [src_b], [dst_b], out=out, in_=in_)


def build_program(debug=False, stop_after=None):
    nc = bass.Bass("TRN2", target_bir_lowering=False)
    B = Builder(nc, debug)
    P = B.P
    A, DMA, MM, TR = B.A, B.DMA, B.MM, B.TR

    def inp(name, shape):
        return B.dram(name, shape, F32, kind="ExternalInput")

    x_in = inp("x", [SEQ, D])
    ctx_in = inp("ctx", [CTX, D])
    cc_in = inp("cc", [128, 8, 2])
    n1g_in = inp("norm1_g", [DEPTH, D])
    n2g_in = inp("norm2_g", [DEPTH, D])
    wmod_in = inp("w_mod", [DEPTH, 128, 8, 6 * D])
    bmod_in = inp("b_mod", [DEPTH, 6 * D])
    win_in = inp("w_in", [DEPTH, 128, 8, N_IN])
    wout_in = inp("w_out", [DEPTH, 128, 8, D])
    w1_in = inp("w_mlp1", [DEPTH, 128, 8, D_FF])
    w2_in = inp("w_mlp2", [DEPTH, 128, 32, D])
    fng_in = inp("final_norm_g", [1, D])
    tri_in = inp("tri", [128, 4, 128])
    mask_in = inp("mask4", [128, 2, 512])
    glawa_in = inp("gla_wa", [DEPTH, 33, 384])
    glang_in = inp("gla_ng", [DEPTH, 96])
    tri1_in = inp("tri1", [128, 4, 128])
    nmask_in = inp("nmask", [128, 4, 128])
    esel_in = inp("esel", [8, 8, 128])
    gmask_in = inp("gmask", [128, 7, 128])
    cmask_in = inp("cmask", [128, 3, 7])
    gdncw_in = inp("gdn_cw", [DEPTH, 1, 7 * 1152])
    gdnpar_in = inp("gdn_par", [DEPTH, 1, 16])
    gdnng_in = inp("gdn_ng", [DEPTH, 96])
    hycw_in = inp("hy_cw", [DEPTH, 1, 3 * 768])
    hycb_in = inp("hy_cb", [DEPTH, 1, 768])
    hyw1_in = inp("hy_w1", [DEPTH, 33, 64])
    hyw2_in = inp("hy_w2", [DEPTH, 64, 64])
    hyw3_in = inp("hy_w3r", [DEPTH, 64, 2, 512])
    hyfb_in = inp("hy_fb", [DEPTH, 64, 4])
    hyd2_in = inp("hy_d2", [DEPTH, 2, 1, 512])
    zpos_in = inp("hy_zpos", [2, 33, 8192])
    dec_in = inp("hy_dec", [2, 8192, 256])
    f1_in = inp("hy_F1", [128, 2, 65])
    mt_in = inp("hy_MT", [128, 65, 5, 128])
    g_in = inp("hy_G", [65, 2, 64])
    out_d = B.dram("out", [SEQ, D], F32, kind="ExternalOutput")

    SK = "ExternalOutput" if debug else "Internal"
    XS = B.dram("XS", [T, D], F32, kind=SK)
    Z = B.dram("Z", [T + 8, N_IN], F32, kind=SK)
    YM = B.dram("YM", [T, D], BF16, kind=SK)
    MOD = B.dram("MOD", [DEPTH, 2, 6 * D], F32, kind=SK)
    GQKV = B.dram("GQKV", [T, 1152], BF16, kind=SK)
    GPAR = B.dram("GPAR", [T, 24], F32, kind=SK)
    HU = B.dram("HU", [T, 768], F32, kind=SK)
    HV = B.dram("HV", [T, 256], BF16, kind=SK)
    HY1 = B.dram("HY1", [T, 256], BF16, kind=SK)
    HF = B.dram("HF", [2, 8192, 256], BF16, kind=SK)
    HC = B.dram("HC", [2, 2, 65, 128, 512], BF16, kind=SK)
    BD = B.dram("BD", [2, 65, 64, 256], BF16, kind=SK)
    CD = B.dram("CD", [2, 65, 64, 256], BF16, kind=SK)
    MTB = B.dram("MTB", [128, 65, 5, 128], BF16, kind=SK)
    dbg = {}

    with Scope(B) as top:
        ident = B.sb(top, "ident", [128, 128], BF16)
        eps_t = B.sb(top, "eps", [128, 1], F32)
        A("pool", "memset", [], [ident], ap=ident[:], constant=1.0)
        A("pool", "affine_select", [ident], [ident], out=ident[:], in_=ident[:], pattern=[[-1, 128]],
          compare_op=ALU.is_equal, fill=0.0, base=0, channel_multiplier=1)
        A("pool", "memset", [], [eps_t], ap=eps_t[:], constant=EPS)

        def x_src(layer, ti):
            if layer == 0:
                if ti < 2:
                    return ctx_in, ctx_in[ti * 128:(ti + 1) * 128, :]
                return x_in, x_in[(ti - 2) * 128:(ti - 1) * 128, :]
            return (XS, ti), XS[ti * 128:(ti + 1) * 128, :]

        def load_weight_bf16(st, name, src_b, src_ap3, kch, ncols, slab=256):
            wt = B.sb(st, name, [128, kch, ncols], BF16)
            with Scope(B) as st2:
                stg = B.ring(st2, name + "stg", [128, kch, slab], F32, 2)
                i = 0
                for c0 in range(0, ncols, slab):
                    c1 = min(ncols, c0 + slab)
                    s = stg[i % 2]
                    DMA([src_b], [s], out=s[:, :, 0:c1 - c0], in_=src_ap3[:, :, c0:c1])
                    eng = ("pool", "dve", "act")[i % 3]
                    B.copy(eng, s, (wt, i), out=wt[:, :, c0:c1], in_=s[:, :, 0:c1 - c0])
                    i += 1
            return wt

        def load_bcast(st, name, src_b, row_ap, n=D):
            t = B.sb(st, name, [128, n], F32)
            DMA([src_b], [t], out=t[:], in_=row_ap.partition_broadcast(128))
            return t

        def rms_rstd(xt, junk, rstd):
            A("act", "activation", [xt], [junk, rstd], out=junk[:], in_=xt[:], func=AF.Square, accum_out=rstd[:])
            A("act", "activation", [rstd, eps_t], [rstd], out=rstd[:], in_=rstd[:], func=AF.Sqrt,
              scale=1.0 / D, bias=eps_t[:, 0:1])
            A("dve", "reciprocal", [rstd], [rstd], out=rstd[:], in_=rstd[:])

        def transpose_tile(src, srcap_fn, tp, dst, dstap):
            for k in range(8):
                TR([src, ident], [tp], out=tp[:, k * 128:(k + 1) * 128], in_=srcap_fn(k), ident=ident[:],
                   inc=(k == 7))
            B.copy("act", tp, dst, out=dstap, in_=tp[:].rearrange("p (k t) -> p k t", k=8))


        identf = B.sb(top, "identf", [128, 128], F32)
        A("pool", "memset", [], [identf], ap=identf[:], constant=1.0)
        A("pool", "affine_select", [identf], [identf], out=identf[:], in_=identf[:], pattern=[[-1, 128]],
          compare_op=ALU.is_equal, fill=0.0, base=0, channel_multiplier=1)

        def gla_mixer(layer, last):
            with Scope(B) as st:
                tri = B.sb(st, "tri", [128, 4, 128], F32)
                msk = B.sb(st, "msk", [128, 2, 512], F32)
                wa = B.sb(st, "wa", [33, 384], F32)
                gng = B.sb(st, "gng", [128, 96], F32)
                DMA([tri_in], [tri], out=tri[:], in_=tri_in[:])
                DMA([mask_in], [msk], out=msk[:], in_=mask_in[:])
                DMA([glawa_in], [wa], out=wa[:], in_=glawa_in[layer])
                DMA([glang_in], [gng], out=gng[:], in_=glang_in[layer:layer + 1, :].partition_broadcast(128))
                O = B.sb(st, "glaO", [128, NT, 384], F32)
                S = [B.sb(st, "glaS%d" % d, [48, 384], F32) for d in range(2)]
                Sb = [B.sb(st, "glaSb%d" % d, [48, 384], BF16) for d in range(2)]
                zg = B.ring(st, "zg", [128, 1184], F32, 2)
                aT = B.ring(st, "aT", [33, 128], F32, 2)
                e1 = B.ring(st, "e1", [128, 192], F32, 2)
                sp = B.ring(st, "sp", [128, 192], F32, 2)
                ebT = B.ring(st, "ebT", [48, 512], F32, 2)
                enbT = B.ring(st, "enbT", [48, 512], F32, 2)
                ecs = B.ring(st, "ecs", [128, 192], F32, 2)
                qkb = B.ring(st, "qkb", [128, 384], BF16, 2)
                qtT = B.ring(st, "qtT", [48, 512], BF16, 2)
                ktT = B.ring(st, "ktT", [48, 512], BF16, 2)
                kp = B.ring(st, "kp", [128, 192], BF16, 2)
                vb = B.ring(st, "vb", [128, 384], BF16, 2)
                atT = B.ring(st, "atT", [128, 512], BF16, 2)
                sg = B.ring(st, "sg", [128, 384], F32, 2)
                ssq = B.ring(st, "ssq", [128, 4], F32, 2)
                yn = B.ring(st, "yn", [128, 384], F32, 2)
                yo = B.ring(st, "yo", [128, 384], BF16, 2)
                junk = B.sb(st, "gjunk", [128, 96], F32)
                p_aT = B.ps(st, "p_aT", [32, 128], F32)
                p_gl = B.ps(st, "p_gl", [128, 192], F32)
                p_bT = B.ps(st, "p_bT", [48, 512], F32)
                p_cs = B.ps(st, "p_cs", [128, 192], F32)
                p_qk = B.ps(st, "p_qk", [48, 1024], BF16)
                p_at = B.ps(st, "p_at", [128, 512], F32)
                p_o = B.ps(st, "p_o", [128, 384], F32)
                p_S = B.ps(st, "p_S", [48, 384], F32)
                for r in range(2):
                    A("pool", "memset", [], [aT[r]], ap=aT[r][:], constant=1.0)
                for d in range(2):
                    A("pool", "memset", [], [S[d]], ap=S[d][:], constant=0.0)
                    A("pool", "memset", [], [Sb[d]], ap=Sb[d][:], constant=0.0)
                    order = list(range(NT)) if d == 0 else [1, 0] + list(range(NT - 1, 1, -1))
                    for it, ti in enumerate(order):
                        r = it % 2
                        z = zg[r]
                        DMA([(Z, ti)], [z], out=z[:], in_=Z[4 + ti * 128:4 + (ti + 1) * 128, GLA0:GLA0 + 1184])
                        TR([z, identf], [p_aT], out=p_aT[:], in_=z[:, 1152:1184], ident=identf[:])
                        B.copy("act", p_aT, aT[r], out=aT[r][0:32, :], in_=p_aT[:])
                        MM([aT[r], wa], [p_gl], p_gl[:], lhsT=aT[r][:], rhs=wa[:, d * 192:(d + 1) * 192], start=True, stop=True)
                        A("act", "activation", [p_gl], [e1[r]], out=e1[r][:], in_=p_gl[:], func=AF.Exp, scale=-1.0)
                        A("act", "activation", [e1[r]], [sp[r]], out=sp[r][:], in_=e1[r][:], func=AF.Ln, bias=1.0)
                        for h in range(4):
                            MM([sp[r], tri], [p_bT], p_bT[:, h * 128:(h + 1) * 128], lhsT=sp[r][:, h * 48:(h + 1) * 48],
                               rhs=tri[:, d, :], start=True, stop=True)
                        MM([sp[r], tri], [p_cs], p_cs[:], lhsT=tri[:, 2 + d, :], rhs=sp[r][:], start=True, stop=True)
                        A("act", "activation", [p_bT], [ebT[r]], out=ebT[r][:], in_=p_bT[:], func=AF.Exp)
                        A("act", "activation", [p_bT], [enbT[r]], out=enbT[r][:], in_=p_bT[:], func=AF.Exp, scale=-1.0)
                        A("act", "activation", [p_cs], [ecs[r]], out=ecs[r][:], in_=p_cs[:], func=AF.Exp)
                        A("act", "activation", [z], [qkb[r]], out=qkb[r][:, 0:192], in_=z[:, 0:192], func=AF.Copy,
                          scale=48.0 ** -0.5)
                        A("pool", "tensor_copy", [z], [qkb[r]], out=qkb[r][:, 192:384], in_=z[:, 192:384])
                        for j in range(8):
                            TR([qkb[r], ident], [p_qk], out=p_qk[:, j * 128:(j + 1) * 128],
                               in_=qkb[r][:, j * 48:(j + 1) * 48], ident=ident[:], inc=(j == 7))
                        A("dve", "tensor_tensor", [p_qk, ebT[r]], [qtT[r]], out=qtT[r][:], in0=p_qk[:, 0:512],
                          in1=ebT[r][:], op=ALU.mult)
                        A("dve", "tensor_tensor", [p_qk, enbT[r]], [ktT[r]], out=ktT[r][:], in0=p_qk[:, 512:1024],
                          in1=enbT[r][:], op=ALU.mult)
                        A("pool", "tensor_tensor", [z, ecs[r]], [kp[r]], out=kp[r][:], in0=z[:, 192:384], in1=ecs[r][:],
                          op=ALU.mult)
                        A("pool", "tensor_copy", [z], [vb[r]], out=vb[r][:], in_=z[:, 384:768])
                        for h in range(4):
                            MM([ktT[r], qtT[r]], [p_at], p_at[:, h * 128:(h + 1) * 128], lhsT=ktT[r][:, h * 128:(h + 1) * 128],
                               rhs=qtT[r][:, h * 128:(h + 1) * 128], start=True, stop=True)
                        A("dve", "tensor_tensor", [p_at, msk], [atT[r]], out=atT[r][:], in0=p_at[:], in1=msk[:, d, :],
                          op=ALU.mult)
                        for h in range(4):
                            MM([qtT[r], Sb[d]], [p_o], p_o[:, h * 96:(h + 1) * 96], lhsT=qtT[r][:, h * 128:(h + 1) * 128],
                               rhs=Sb[d][:, h * 96:(h + 1) * 96], start=True, stop=False)
                            MM([atT[r], vb[r]], [p_o], p_o[:, h * 96:(h + 1) * 96], lhsT=atT[r][:, h * 128:(h + 1) * 128],
                               rhs=vb[r][:, h * 96:(h + 1) * 96], start=False, stop=True)
                        if d == 0:
                            B.copy("act", p_o, (O, ti), out=O[:, ti, :], in_=p_o[:])
                        else:
                            A("dve", "tensor_tensor", [p_o, (O, ti)], [(O, ti)], out=O[:, ti, :], in0=p_o[:],
                              in1=O[:, ti, :], op=ALU.add)
                        for h in range(4):
                            MM([kp[r], vb[r]], [p_S], p_S[:, h * 96:(h + 1) * 96], lhsT=kp[r][:, h * 48:(h + 1) * 48],
                               rhs=vb[r][:, h * 96:(h + 1) * 96], start=True, stop=True)
                        col = 127 if d == 0 else 0
                        for h in range(4):
                            A("dve", "scalar_tensor_tensor", [S[d], ebT[r], p_S], [S[d]], out=S[d][:, h * 96:(h + 1) * 96],
                              in0=S[d][:, h * 96:(h + 1) * 96], scalar=ebT[r][:, h * 128 + col:h * 128 + col + 1],
                              in1=p_S[:, h * 96:(h + 1) * 96], op0=ALU.mult, op1=ALU.add)
                        A("pool", "tensor_copy", [S[d]], [Sb[d]], out=Sb[d][:], in_=S[d][:])
                        if d == 1 and not (last and ti < 2):
                            A("act", "activation", [z], [sg[r]], out=sg[r][:], in_=z[:, 768:1152], func=AF.Silu)
                            for h in range(4):
                                A("act", "activation", [(O, ti)], [junk, (ssq[r], h)], out=junk[:],
                                  in_=O[:, ti, h * 96:(h + 1) * 96], func=AF.Square, accum_out=ssq[r][:, h:h + 1])
                            A("act", "activation", [ssq[r], eps_t], [ssq[r]], out=ssq[r][:], in_=ssq[r][:], func=AF.Sqrt,
                              scale=1.0 / 96, bias=eps_t[:, 0:1])
                            A("dve", "reciprocal", [ssq[r]], [ssq[r]], out=ssq[r][:], in_=ssq[r][:])
                            for h in range(4):
                                A("dve", "scalar_tensor_tensor", [(O, ti), ssq[r], gng], [(yn[r], h)],
                                  out=yn[r][:, h * 96:(h + 1) * 96], in0=O[:, ti, h * 96:(h + 1) * 96],
                                  scalar=ssq[r][:, h:h + 1], in1=gng[:], op0=ALU.mult, op1=ALU.mult)
                            A("pool", "tensor_tensor", [yn[r], sg[r]], [yo[r]], out=yo[r][:], in0=yn[r][:], in1=sg[r][:],
                              op=ALU.mult)
                            DMA([yo[r]], [(YM, ("gla", ti))], out=YM[ti * 128:(ti + 1) * 128, 256:640], in_=yo[r][:])


        def zero_pad_rows():
            with Scope(B) as st:
                zr = B.sb(st, "zrow", [4, N_IN], F32)
                A("pool", "memset", [], [zr], ap=zr[:], constant=0.0)
                DMA([zr], [(Z, "pad0")], out=Z[0:4, :], in_=zr[:])
                DMA([zr], [(Z, "pad1")], out=Z[4 + T:8 + T, :], in_=zr[:])

        def conv_taps(zsh, acc, tmp, wk, cm, tt, ntap, width, zcol0, ti):
            half = ntap // 2
            for k in range(ntap):
                sft = k - half
                zs = zsh[k % len(zsh)]
                r0 = 4 + ti * 128 + sft
                DMA([Z], [zs], out=zs[:, 0:width], in_=Z[r0:r0 + 128, zcol0:zcol0 + width])
                dst = acc if k == 0 else tmp
                A("dve", "scalar_tensor_tensor", [zs, cm, wk], [dst], out=dst[:, 0:width], in0=zs[:, 0:width],
                  scalar=cm[:, tt, 3 + sft:4 + sft], in1=wk[:, k, 0:width], op0=ALU.mult, op1=ALU.mult)
                if k > 0:
                    A("pool", "tensor_tensor", [acc, tmp], [acc], out=acc[:, 0:width], in0=acc[:, 0:width],
                      in1=tmp[:, 0:width], op=ALU.add)

        def gdn_prep(layer):
            with Scope(B) as st:
                wk = B.sb(st, "gwk", [128, 7, 1152], F32)
                cm = B.sb(st, "gcm", [128, 3, 7], F32)
                par = B.sb(st, "gpar", [128, 16], F32)
                negA = B.sb(st, "gnegA", [128, 8], F32)
                DMA([gdncw_in], [wk], out=wk[:].rearrange("p k c -> p (k c)"),
                    in_=gdncw_in[layer].partition_broadcast(128))
                DMA([cmask_in], [cm], out=cm[:], in_=cmask_in[:])
                DMA([gdnpar_in], [par], out=par[:], in_=gdnpar_in[layer].partition_broadcast(128))
                A("act", "activation", [par], [negA], out=negA[:], in_=par[:, 0:8], func=AF.Exp)
                A("act", "activation", [negA], [negA], out=negA[:], in_=negA[:], func=AF.Copy, scale=-1.0)
                zsh = B.ring(st, "gzsh", [128, 1152], F32, 3)
                acc = B.ring(st, "gacc", [128, 1152], F32, 2)
                tmp = B.sb(st, "gtmp", [128, 1152], F32)
                u = B.ring(st, "gu", [128, 1152], F32, 2)
                sq = B.sb(st, "gsq", [128, 768], F32)
                ssq = B.ring(st, "gssq", [128, 8], F32, 2)
                ob = B.ring(st, "gob", [128, 1152], BF16, 2)
                zp = B.ring(st, "gzp", [128, 16], F32, 2)
                t8 = B.ring(st, "gt8", [128, 16], F32, 2)
                gp = B.ring(st, "ggp", [128, 24], F32, 2)
                for ti in range(NT):
                    r = ti % 2
                    tt = 0 if ti >= 2 else (1 + ti)
                    conv_taps(zsh, acc[r], tmp, wk, cm, tt, 7, 1152, GDN0, ti)
                    A("act", "activation", [acc[r]], [u[r]], out=u[r][:], in_=acc[r][:], func=AF.Silu)
                    A("pool", "tensor_tensor", [u[r]], [sq], out=sq[:], in0=u[r][:, 0:768], in1=u[r][:, 0:768], op=ALU.mult)
                    A("dve", "tensor_reduce", [sq], [ssq[r]], out=ssq[r][:], in_=sq[:].rearrange("p (h d) -> p h d", d=96),
                      axis=mybir.AxisListType.X, op=ALU.add)
                    A("act", "activation", [ssq[r], eps_t], [ssq[r]], out=ssq[r][:], in_=ssq[r][:], func=AF.Sqrt,
                      bias=eps_t[:, 0:1])
                    A("dve", "reciprocal", [ssq[r]], [ssq[r]], out=ssq[r][:], in_=ssq[r][:])
                    A("act", "activation", [ssq[r]], [ssq[r]], out=ssq[r][:, 0:4], in_=ssq[r][:, 0:4], func=AF.Copy,
                      scale=96.0 ** -0.5)
                    A("dve", "tensor_tensor", [u[r], ssq[r]], [ob[r]],
                      out=ob[r][:, 0:768].rearrange("p (h d) -> p h d", d=96),
                      in0=u[r][:, 0:768].rearrange("p (h d) -> p h d", d=96),
                      in1=ssq[r][:, 0:8].unsqueeze(2).to_broadcast([128, 8, 96]), op=ALU.mult)
                    A("pool", "tensor_copy", [u[r]], [ob[r]], out=ob[r][:, 768:1152], in_=u[r][:, 768:1152])
                    DMA([ob[r]], [(GQKV, ti)], out=GQKV[ti * 128:(ti + 1) * 128, :], in_=ob[r][:])
                    DMA([(Z, ti)], [zp[r]], out=zp[r][:], in_=Z[4 + ti * 128:4 + (ti + 1) * 128, GDN0 + 1536:GDN0 + 1552])
                    A("dve", "tensor_tensor", [zp[r], par], [t8[r]], out=t8[r][:, 0:8], in0=zp[r][:, 0:8], in1=par[:, 8:16],
                      op=ALU.add)
                    A("act", "activation", [t8[r]], [t8[r]], out=t8[r][:, 0:8], in_=t8[r][:, 0:8], func=AF.Exp)
                    A("act", "activation", [t8[r]], [t8[r]], out=t8[r][:, 0:8], in_=t8[r][:, 0:8], func=AF.Ln, bias=1.0)
                    A("dve", "tensor_tensor", [t8[r], negA], [gp[r]], out=gp[r][:, 0:8], in0=t8[r][:, 0:8], in1=negA[:],
                      op=ALU.mult)
                    A("act", "activation", [zp[r]], [t8[r]], out=t8[r][:, 8:16], in_=zp[r][:, 8:16], func=AF.Exp, scale=-1.0)
                    A("act", "activation", [t8[r]], [t8[r]], out=t8[r][:, 8:16], in_=t8[r][:, 8:16], func=AF.Ln, bias=1.0)
                    A("act", "activation", [t8[r]], [gp[r]], out=gp[r][:, 8:16], in_=t8[r][:, 8:16], func=AF.Copy, scale=-1.0)
                    A("act", "activation", [gp[r]], [gp[r]], out=gp[r][:, 16:24], in_=gp[r][:, 8:16], func=AF.Exp)
                    DMA([gp[r]], [(GPAR, ti)], out=GPAR[ti * 128:(ti + 1) * 128, :], in_=gp[r][:])

        def gdn_mixer(layer, last):
            gdn_prep(layer)
            if stop_after == ("GDNPREP", layer):
                return
            with Scope(B) as st:
                tri1 = B.sb(st, "tri1", [128, 4, 128], F32)
                nmk = B.sb(st, "nmk", [128, 4, 128], F32)
                esel = B.sb(st, "esel", [8, 8, 128], F32)
                id4 = B.sb(st, "id4", [128, 512], BF16)
                gng = B.sb(st, "dng", [128, 96], F32)
                DMA([tri1_in], [tri1], out=tri1[:], in_=tri1_in[:])
                DMA([nmask_in], [nmk], out=nmk[:], in_=nmask_in[:])
                DMA([esel_in], [esel], out=esel[:], in_=esel_in[:])
                DMA([gdnng_in], [gng], out=gng[:], in_=gdnng_in[layer:layer + 1, :].partition_broadcast(128))
                for h in range(4):
                    A("pool", "tensor_copy", [ident], [id4], out=id4[:, h * 128:(h + 1) * 128], in_=ident[:])
                O = B.sb(st, "gdnO", [128, NT, 384], F32)
                Sd = [B.sb(st, "gdnS%d" % d, [96, 384], F32) for d in range(2)]
                Sb = [B.sb(st, "gdnSb%d" % d, [96, 384], BF16) for d in range(2)]
                R2 = lambda name, shape, dt: B.ring(st, name, shape, dt, 2)
                qkv = R2("dqkv", [128, 1152], BF16)
                gp = R2("dgp", [128, 24], F32)
                gc = R2("dgc", [128, 8], F32)
                gct = R2("dgct", [128, 8], F32)
                gT = R2("dgT", [8, 128], F32)
                ngT = R2("dngT", [8, 128], F32)
                esc = R2("desc", [128, 8], F32)
                DT = R2("dDT", [128, 512], F32)
                DsB = R2("dDsB", [128, 512], F32)
                egT = R2("degT", [96, 512], F32)
                kT = R2("dkT", [96, 512], BF16)
                nkT = R2("dnkT", [96, 512], BF16)
                qT = R2("dqT", [96, 512], BF16)
                qtT = R2("dqtT", [96, 512], BF16)
                Am = R2("dA", [128, 512], BF16)
                Bm = R2("dB", [128, 512], BF16)
                Ball = R2("dBall", [128, 7, 512], BF16)
                Aall = R2("dAall", [128, 7, 512], BF16)
                P1s = R2("dP1s", [128, 512], BF16)
                P1t = R2("dP1t", [128, 512], BF16)
                gmf = B.sb(st, "gmf", [128, 7, 128], F32)
                gmk = B.sb(st, "gmk", [128, 7, 512], BF16)
                DMA([gmask_in], [gmf], out=gmf[:], in_=gmask_in[:])
                for h in range(4):
                    A("pool", "tensor_copy", [gmf], [gmk], out=gmk[:, :, h * 128:(h + 1) * 128], in_=gmf[:])
                atT = R2("datT", [128, 512], BF16)
                ST = B.ring(st, "dST", [128, 512], BF16, 3)
                Y = B.ring(st, "dY", [128, 512], BF16, 3)
                nWT = R2("dnWT", [96, 512], BF16)
                rw = R2("drw", [128, 384], BF16)
                ru = R2("dru", [128, 384], BF16)
                kp = R2("dkp", [128, 384], BF16)
                vn = R2("dvn", [128, 384], BF16)
                zgate = R2("dzg", [128, 384], F32)
                sg = R2("dsg", [128, 384], F32)
                ssq = R2("dssq", [128, 4], F32)
                yn = R2("dyn", [128, 384], F32)
                yo = R2("dyo", [128, 384], BF16)
                junk = B.sb(st, "djunk", [128, 96], F32)
                pf = B.ring(st, "dpf", [128, 512], F32, 6, psum=True)
                pb = B.ring(st, "dpb", [128, 1024], BF16, 2, psum=True)
                cnt = {"f": 0, "b": 0}

                def PF():
                    cnt["f"] += 1
                    return pf[cnt["f"] % 6]

                def PB():
                    cnt["b"] += 1
                    return pb[cnt["b"] % 2]

                sidx = 0
                for d in range(2):
                    A("pool", "memset", [], [Sd[d]], ap=Sd[d][:], constant=0.0)
                    A("pool", "memset", [], [Sb[d]], ap=Sb[d][:], constant=0.0)
                    if B.debug and d == 1:
                        dOf = B.dram("dbg_Of", [128, NT, 384], F32, kind="ExternalOutput")
                        DMA([O], [dOf], out=dOf[:], in_=O[:])
                        dSf = B.dram("dbg_Sf", [96, 384], F32, kind="ExternalOutput")
                        DMA([Sd[0]], [dSf], out=dSf[:], in_=Sd[0][:])
                    order = list(range(NT)) if d == 0 else [1, 0] + list(range(NT - 1, 1, -1))
                    gstop = stop_after[1] if (stop_after and stop_after[0] == "GDN") else 99
                    if gstop < 99:
                        order = order[:3]
                    col = 127 if d == 0 else 0
                    for it, ti in enumerate(order):
                        r = it % 2
                        DMA([(GQKV, ti)], [qkv[r]], out=qkv[r][:], in_=GQKV[ti * 128:(ti + 1) * 128, :])
                        DMA([(GPAR, ti)], [gp[r]], out=gp[r][:], in_=GPAR[ti * 128:(ti + 1) * 128, :])
                        g_d = gp[r][:, d * 4:(d + 1) * 4]
                        p = PF()
                        MM([tri1, gp[r]], [p], p[:, 0:4], lhsT=tri1[:, d, :], rhs=g_d, start=True, stop=True)
                        MM([tri1, gp[r]], [p], p[:, 4:8], lhsT=tri1[:, 2 + d, :], rhs=g_d, start=True, stop=True)
                        B.copy("act", p, gc[r], out=gc[r][:], in_=p[:, 0:8])
                        A("pool", "tensor_copy", [gc[r]], [gct[r]], out=gct[r][:, 0:4], in_=gc[r][:, 0:4])
                        A("dve", "tensor_tensor", [gc[r], gp[r]], [gct[r]], out=gct[r][:, 4:8], in0=gc[r][:, 0:4],
                          in1=gp[r][:, 8 + d * 4:12 + d * 4], op=ALU.add)
                        A("act", "activation", [gct[r]], [esc[r]], out=esc[r][:, 0:4], in_=gct[r][:, 4:8], func=AF.Exp)
                        A("act", "activation", [gc[r]], [esc[r]], out=esc[r][:, 4:8], in_=gc[r][:, 4:8], func=AF.Exp)
                        p = PF()
                        TR([gct[r], identf], [p], out=p[0:8, 0:128], in_=gct[r][:], ident=identf[:])
                        B.copy("act", p, gT[r], out=gT[r][:], in_=p[0:8, 0:128])
                        A("act", "activation", [p], [ngT[r]], out=ngT[r][:], in_=p[0:8, 0:128], func=AF.Copy, scale=-1.0)
                        if gstop <= 1:
                            continue
                        p1 = PF()
                        p2 = PF()
                        p3 = PF()
                        for h in range(4):
                            hs = slice(h * 128, (h + 1) * 128)
                            MM([esel, gT[r]], [p1], p1[:, hs], lhsT=esel[:, h, :], rhs=gT[r][:], start=True, stop=False)
                            MM([ngT[r], esel], [p1], p1[:, hs], lhsT=ngT[r][:], rhs=esel[:, h, :], start=False, stop=False)
                            MM([identf, nmk], [p1], p1[:, hs], lhsT=identf[:], rhs=nmk[:, d, :], start=False, stop=True)
                            MM([gT[r], esel], [p2], p2[:, hs], lhsT=gT[r][:], rhs=esel[:, 4 + h, :], start=True, stop=False)
                            MM([esel, ngT[r]], [p2], p2[:, hs], lhsT=esel[:, h, :], rhs=ngT[r][:], start=False, stop=False)
                            MM([identf, nmk], [p2], p2[:, hs], lhsT=identf[:], rhs=nmk[:, 2 + d, :], start=False, stop=True)
                            MM([esel, gT[r]], [p3], p3[0:96, hs], lhsT=esel[:, h, 0:96], rhs=gT[r][:], start=True, stop=True)
                        A("act", "activation", [p1], [DT[r]], out=DT[r][:], in_=p1[:], func=AF.Exp)
                        A("act", "activation", [p2], [DsB[r]], out=DsB[r][:], in_=p2[:], func=AF.Exp)
                        A("act", "activation", [p3], [egT[r]], out=egT[r][:], in_=p3[0:96, :], func=AF.Exp)
                        if gstop <= 2:
                            continue
                        pq = PB()
                        for j in range(8):
                            TR([qkv[r], ident], [pq], out=pq[0:96, j * 128:(j + 1) * 128], in_=qkv[r][:, j * 96:(j + 1) * 96],
                               ident=ident[:], inc=(j == 7))
                        B.copy("dve", pq, qT[r], out=qT[r][:], in_=pq[0:96, 0:512])
                        A("dve", "tensor_tensor", [pq, egT[r]], [qtT[r]], out=qtT[r][:], in0=pq[0:96, 0:512], in1=egT[r][:],
                          op=ALU.mult)
                        B.copy("dve", pq, kT[r], out=kT[r][:], in_=pq[0:96, 512:1024])
                        if gstop <= 3:
                            continue
                        pk = PF()
                        pa = PF()
                        for h in range(4):
                            hs = slice(h * 128, (h + 1) * 128)
                            MM([kT[r]], [pk], pk[:, hs], lhsT=kT[r][:, hs], rhs=kT[r][:, hs], start=True, stop=True)
                            MM([kT[r], qT[r]], [pa], pa[:, hs], lhsT=kT[r][:, hs], rhs=qT[r][:, hs], start=True, stop=True)
                        A("dve", "scalar_tensor_tensor", [pk, DsB[r]], [Am[r]], out=Am[r][:], in0=pk[:], scalar=-1.0,
                          in1=DsB[r][:], op0=ALU.mult, op1=ALU.mult)
                        A("dve", "tensor_tensor", [pa, DT[r]], [atT[r]], out=atT[r][:], in0=pa[:], in1=DT[r][:], op=ALU.mult)
                        if gstop <= 4:
                            continue
                        pt = PB()
                        for h in range(4):
                            TR([Am[r], ident], [pt], out=pt[:, h * 128:(h + 1) * 128], in_=Am[r][:, h * 128:(h + 1) * 128],
                               ident=ident[:], inc=(h == 3))
                        B.copy("dve", pt, Bm[r], out=Bm[r][:], in_=pt[:, 0:512])
                        A("dve", "tensor_tensor", [gmk, Bm[r]], [Ball[r]], out=Ball[r][:], in0=gmk[:],
                          in1=Bm[r][:].unsqueeze(1).to_broadcast([128, 7, 512]), op=ALU.mult)
                        A("pool", "tensor_tensor", [gmk, Am[r]], [Aall[r]], out=Aall[r][:], in0=gmk[:],
                          in1=Am[r][:].unsqueeze(1).to_broadcast([128, 7, 512]), op=ALU.mult)
                        y_cur = Y[sidx % 3]
                        yt_cur = ST[sidx % 3]
                        sidx += 1
                        A("pool", "tensor_tensor", [Ball[r], id4], [y_cur], out=y_cur[:], in0=Ball[r][:, 6, :], in1=id4[:], op=ALU.add)
                        A("pool", "tensor_tensor", [Aall[r], id4], [yt_cur], out=yt_cur[:], in0=Aall[r][:, 6, :], in1=id4[:], op=ALU.add)
                        for lvl in range(6):
                            fin = lvl == 5
                            y_new = Y[sidx % 3]
                            yt_new = ST[sidx % 3]
                            sidx += 1
                            p1 = PF()
                            for h in range(4):
                                hs = slice(h * 128, (h + 1) * 128)
                                MM([Aall[r], y_cur], [p1], p1[:, hs], lhsT=Aall[r][:, lvl, hs], rhs=y_cur[:, hs], start=True, stop=True)
                            B.copy("act", p1, P1s[r], out=P1s[r][:], in_=p1[:])
                            if not fin:
                                p1t = PF()
                                for h in range(4):
                                    hs = slice(h * 128, (h + 1) * 128)
                                    MM([Ball[r], yt_cur], [p1t], p1t[:, hs], lhsT=Ball[r][:, lvl, hs], rhs=yt_cur[:, hs], start=True, stop=True)
                                B.copy("act", p1t, P1t[r], out=P1t[r][:], in_=p1t[:])
                            p2 = PF()
                            for h in range(4):
                                hs = slice(h * 128, (h + 1) * 128)
                                MM([yt_cur, P1s[r]], [p2], p2[:, hs], lhsT=yt_cur[:, hs], rhs=P1s[r][:, hs], start=True, stop=True)
                            A("dve", "tensor_tensor", [p2, y_cur], [y_new], out=y_new[:], in0=p2[:], in1=y_cur[:], op=ALU.add)
                            if not fin:
                                p2t = PF()
                                for h in range(4):
                                    hs = slice(h * 128, (h + 1) * 128)
                                    MM([y_cur, P1t[r]], [p2t], p2t[:, hs], lhsT=y_cur[:, hs], rhs=P1t[r][:, hs], start=True, stop=True)
                                A("dve", "tensor_tensor", [p2t, yt_cur], [yt_new], out=yt_new[:], in0=p2t[:], in1=yt_cur[:], op=ALU.add)
                            y_cur, yt_cur = y_new, yt_new
                        if B.debug and d == 0 and it == 0:
                            for nm, tl, shp, dt_ in (("dDT", DT[r], [128, 512], F32), ("dDsB", DsB[r], [128, 512], F32),
                                                     ("dAm", Am[r], [128, 512], BF16), ("dY", y_cur, [128, 512], BF16),
                                                     ("datT", atT[r], [128, 512], BF16), ("dqtT", qtT[r], [96, 512], BF16),
                                                     ("dkT", kT[r], [96, 512], BF16), ("degT", egT[r], [96, 512], F32),
                                                     ("dgc", gc[r], [128, 8], F32), ("desc", esc[r], [128, 8], F32)):
                                dd_ = B.dram("dbg_" + nm, shp, dt_, kind="ExternalOutput")
                                DMA([tl], [dd_], out=dd_[:], in_=tl[:])
                        k3 = qkv[r][:, 384:768].rearrange("p (h d) -> p h d", d=96)
                        v3 = qkv[r][:, 768:1152].rearrange("p (h d) -> p h d", d=96)
                        bc = lambda ap: ap.unsqueeze(2).to_broadcast([128, 4, 96])
                        A("dve", "tensor_tensor", [qkv[r], esc[r]], [rw[r]], out=rw[r][:].rearrange("p (h d) -> p h d", d=96),
                          in0=k3, in1=bc(esc[r][:, 0:4]), op=ALU.mult)
                        A("dve", "tensor_tensor", [qkv[r], gp[r]], [ru[r]], out=ru[r][:].rearrange("p (h d) -> p h d", d=96),
                          in0=v3, in1=bc(gp[r][:, 16 + d * 4:20 + d * 4]), op=ALU.mult)
                        A("dve", "tensor_tensor", [qkv[r], esc[r]], [kp[r]], out=kp[r][:].rearrange("p (h d) -> p h d", d=96),
                          in0=k3, in1=bc(esc[r][:, 4:8]), op=ALU.mult)
                        pw = PF()
                        for h in range(4):
                            MM([rw[r], y_cur], [pw], pw[0:96, h * 128:(h + 1) * 128], lhsT=rw[r][:, h * 96:(h + 1) * 96],
                               rhs=y_cur[:, h * 128:(h + 1) * 128], start=True, stop=True)
                        A("act", "activation", [pw], [nWT[r]], out=nWT[r][:], in_=pw[0:96, :], func=AF.Copy, scale=-1.0)
                        if gstop <= 7:
                            continue
                        pv = PF()
                        for h in range(4):
                            MM([y_cur, ru[r]], [pv], pv[:, h * 96:(h + 1) * 96], lhsT=y_cur[:, h * 128:(h + 1) * 128],
                               rhs=ru[r][:, h * 96:(h + 1) * 96], start=True, stop=False)
                            MM([nWT[r], Sb[d]], [pv], pv[:, h * 96:(h + 1) * 96], lhsT=nWT[r][:, h * 128:(h + 1) * 128],
                               rhs=Sb[d][:, h * 96:(h + 1) * 96], start=False, stop=True)
                        B.copy("act", pv, vn[r], out=vn[r][:], in_=pv[:, 0:384])
                        if B.debug and d == 0 and it == 0:
                            for nm, tl, shp, dt_ in (("dvn", vn[r], [128, 384], BF16), ("dnWT", nWT[r], [96, 512], BF16),
                                                     ("drw", rw[r], [128, 384], BF16), ("dru", ru[r], [128, 384], BF16),
                                                     ("dkp", kp[r], [128, 384], BF16)):
                                dd_ = B.dram("dbg_" + nm, shp, dt_, kind="ExternalOutput")
                                DMA([tl], [dd_], out=dd_[:], in_=tl[:])
                        po = PF()
                        for h in range(4):
                            MM([qtT[r], Sb[d]], [po], po[:, h * 96:(h + 1) * 96], lhsT=qtT[r][:, h * 128:(h + 1) * 128],
                               rhs=Sb[d][:, h * 96:(h + 1) * 96], start=True, stop=False)
                            MM([atT[r], vn[r]], [po], po[:, h * 96:(h + 1) * 96], lhsT=atT[r][:, h * 128:(h + 1) * 128],
                               rhs=vn[r][:, h * 96:(h + 1) * 96], start=False, stop=True)
                        if d == 0:
                            B.copy("act", po, (O, ti), out=O[:, ti, :], in_=po[:, 0:384])
                        else:
                            A("dve", "tensor_tensor", [po, (O, ti)], [(O, ti)], out=O[:, ti, :], in0=po[:, 0:384],
                              in1=O[:, ti, :], op=ALU.add)
                        pS = PF()
                        for h in range(4):
                            MM([kp[r], vn[r]], [pS], pS[0:96, h * 96:(h + 1) * 96], lhsT=kp[r][:, h * 96:(h + 1) * 96],
                               rhs=vn[r][:, h * 96:(h + 1) * 96], start=True, stop=True)
                        for h in range(4):
                            A("dve", "scalar_tensor_tensor", [Sd[d], egT[r], pS], [Sd[d]], out=Sd[d][:, h * 96:(h + 1) * 96],
                              in0=Sd[d][:, h * 96:(h + 1) * 96], scalar=egT[r][:, h * 128 + col:h * 128 + col + 1],
                              in1=pS[0:96, h * 96:(h + 1) * 96], op0=ALU.mult, op1=ALU.add)
                        A("pool", "tensor_copy", [Sd[d]], [Sb[d]], out=Sb[d][:], in_=Sd[d][:])
                        if d == 1 and not (last and ti < 2):
                            DMA([(Z, ti)], [zgate[r]], out=zgate[r][:],
                                in_=Z[4 + ti * 128:4 + (ti + 1) * 128, GDN0 + 1152:GDN0 + 1536])
                            A("act", "activation", [zgate[r]], [sg[r]], out=sg[r][:], in_=zgate[r][:], func=AF.Silu)
                            for h in range(4):
                                A("act", "activation", [(O, ti)], [junk, (ssq[r], h)], out=junk[:],
                                  in_=O[:, ti, h * 96:(h + 1) * 96], func=AF.Square, accum_out=ssq[r][:, h:h + 1])
                            A("act", "activation", [ssq[r], eps_t], [ssq[r]], out=ssq[r][:], in_=ssq[r][:], func=AF.Sqrt,
                              scale=1.0 / 96, bias=eps_t[:, 0:1])
                            A("dve", "reciprocal", [ssq[r]], [ssq[r]], out=ssq[r][:], in_=ssq[r][:])
                            for h in range(4):
                                A("dve", "scalar_tensor_tensor", [(O, ti), ssq[r], gng], [(yn[r], h)],
                                  out=yn[r][:, h * 96:(h + 1) * 96], in0=O[:, ti, h * 96:(h + 1) * 96],
                                  scalar=ssq[r][:, h:h + 1], in1=gng[:], op0=ALU.mult, op1=ALU.mult)
                            A("pool", "tensor_tensor", [yn[r], sg[r]], [yo[r]], out=yo[r][:], in0=yn[r][:], in1=sg[r][:],
                              op=ALU.mult)
                            DMA([yo[r]], [(YM, ("gdn", ti))], out=YM[ti * 128:(ti + 1) * 128, 640:1024], in_=yo[r][:])
                if B.debug:
                    dOa = B.dram("dbg_Oa", [128, NT, 384], F32, kind="ExternalOutput")
                    DMA([O], [dOa], out=dOa[:], in_=O[:])


        PI = math.pi

        def hy_cast_tables():
            with Scope(B) as st:
                stg = B.ring(st, "mtstg", [128, 5, 640], F32, 2)
                stb = B.ring(st, "mtstb", [128, 5, 640], BF16, 2)
                for i in range(13):
                    r = i % 2
                    DMA([mt_in], [stg[r]], out=stg[r][:], in_=mt_in[:, i * 5:(i + 1) * 5].rearrange("p f v m -> p f (v m)"))
                    B.copy(("dve", "pool")[r], stg[r], stb[r], out=stb[r][:], in_=stg[r][:])
                    DMA([stb[r]], [(MTB, i)], out=MTB[:, i * 5:(i + 1) * 5].rearrange("p f v m -> p f (v m)"), in_=stb[r][:])

        def hy_conv_pass(layer):
            with Scope(B) as st:
                wk = B.sb(st, "hwk", [128, 3, 768], F32)
                cb = B.sb(st, "hcb", [128, 768], F32)
                cm = B.sb(st, "hcm", [128, 3, 7], F32)
                DMA([hycw_in], [wk], out=wk[:].rearrange("p k c -> p (k c)"), in_=hycw_in[layer].partition_broadcast(128))
                DMA([hycb_in], [cb], out=cb[:], in_=hycb_in[layer].partition_broadcast(128))
                DMA([cmask_in], [cm], out=cm[:], in_=cmask_in[:])
                zsh = B.ring(st, "hzsh", [128, 768], F32, 3)
                acc = B.ring(st, "hacc", [128, 768], F32, 2)
                tmp = B.sb(st, "htmp", [128, 768], F32)
                vb = B.ring(st, "hvb", [128, 256], BF16, 2)
                for ti in range(NT):
                    r = ti % 2
                    tt = 0 if ti >= 2 else (1 + ti)
                    conv_taps(zsh, acc[r], tmp, wk, cm, tt, 3, 768, 0, ti)
                    A("pool", "tensor_tensor", [acc[r], cb], [acc[r]], out=acc[r][:], in0=acc[r][:], in1=cb[:], op=ALU.add)
                    A("pool", "tensor_copy", [acc[r]], [vb[r]], out=vb[r][:], in_=acc[r][:, 0:256])
                    DMA([acc[r]], [(HU, ti)], out=HU[ti * 128:(ti + 1) * 128, :], in_=acc[r][:])
                    DMA([vb[r]], [(HV, ti)], out=HV[ti * 128:(ti + 1) * 128, :], in_=vb[r][:])

        def hy_load_small(st, layer):
            f1f = B.sb(st, "f1f", [128, 2, 65], F32)
            f1b = B.sb(st, "f1b", [128, 2, 65], BF16)
            DMA([f1_in], [f1f], out=f1f[:], in_=f1_in[:])
            A("pool", "tensor_copy", [f1f], [f1b], out=f1b[:], in_=f1f[:])
            return f1b

        def hy_stage1(src_b, src_ap, nt2, f1b):
            with Scope(B) as st:
                Ld = B.sb(st, "Ld", [128, 16384], BF16)
                bt = B.ring(st, "bt", [65, 2, 4096], BF16, 2)
                pp = B.ring(st, "s1p", [65, 512], F32, 4, psum=True)
                DMA([src_b], [Ld], out=Ld[0:nt2, :], in_=src_ap.rearrange("(a b) c -> a (b c)", b=64))
                for g in range(4):
                    b_ = bt[g % 2]
                    for s8 in range(8):
                        sl = slice((g * 8 + s8) * 512, (g * 8 + s8 + 1) * 512)
                        for ri in range(2):
                            p = pp[(s8 * 2 + ri) % 4]
                            MM([f1b, Ld], [p], p[:], lhsT=f1b[0:nt2, ri, :], rhs=Ld[0:nt2, sl], start=True, stop=True)
                            B.copy("act" if ri else "dve", p, (b_, (ri, s8)), out=b_[:, ri, s8 * 512:(s8 + 1) * 512], in_=p[:])
                    for ri in range(2):
                        DMA([b_], [(BD, (ri, g))], out=BD[ri, :, g * 16:(g + 1) * 16, :].rearrange("f t c -> f (t c)"),
                            in_=b_[:, ri, :])

        def hy_stage2(variants, consume):
            with Scope(B) as st:
                R = B.sb(st, "Rall", [128, 65, 256], BF16)
                mab = B.sb(st, "mab", [128, 65, 2, 128], BF16)
                for ri in range(2):
                    DMA([BD], [(R, ri)], out=R[ri * 64:(ri + 1) * 64], in_=BD[ri].rearrange("f t c -> t f c"))
                for j, v in enumerate(variants):
                    DMA([MTB], [(mab, j)], out=mab[:, :, j, :], in_=MTB[:, :, v, :])
                px = B.ring(st, "s2p", [128, 512], F32, 2, psum=True)
                state = consume(st, None, None)
                for f2 in range(65):
                    p = px[f2 % 2]
                    for j in range(2):
                        MM([R, mab], [p], p[:, j * 256:(j + 1) * 256], lhsT=mab[:, f2, j, :], rhs=R[:, f2, :], start=True, stop=True)
                    consume(st, f2, p, state)
                consume(st, 65, None, state)

        def hy_filters(layer, variant):
            with Scope(B) as st:
                w1 = B.sb(st, "fw1", [33, 64], F32)
                w2 = B.sb(st, "fw2", [64, 64], F32)
                w3 = B.sb(st, "fw3", [64, 2, 512], F32)
                fb = B.sb(st, "ffb", [64, 4], F32)
                npi = B.sb(st, "npi", [64, 1], F32)
                DMA([hyw1_in], [w1], out=w1[:], in_=hyw1_in[layer])
                DMA([hyw2_in], [w2], out=w2[:], in_=hyw2_in[layer])
                DMA([hyw3_in], [w3], out=w3[:], in_=hyw3_in[layer])
                DMA([hyfb_in], [fb], out=fb[:], in_=hyfb_in[layer])
                A("pool", "memset", [], [npi], ap=npi[:], constant=PI / 2)
                cs = B.ring(st, "fcs", [64, 512], F32, 2)
                s2 = B.ring(st, "fs2", [64, 512], F32, 2)
                zp = B.ring(st, "fzp", [33, 512], F32, 2)
                arg = B.ring(st, "farg", [64, 512], F32, 2)
                h1 = B.ring(st, "fh1", [64, 512], F32, 2)
                h2 = B.ring(st, "fh2", [64, 512], F32, 2)
                dc = B.ring(st, "fdc", [128, 256], F32, 2)
                hf = B.ring(st, "fhf", [128, 2, 256], BF16, 2)
                pm = B.ring(st, "fpm", [64, 512], F32, 2, psum=True)
                po = B.ring(st, "fpo", [128, 512], F32, 2, psum=True)
                for pb in range(16):
                    r = pb % 2
                    DMA([zpos_in], [zp[r]], out=zp[r][:], in_=zpos_in[variant, :, pb * 512:(pb + 1) * 512])
                    src = zp[r]
                    for li, (w, hh) in enumerate(((w1, h1[r]), (w2, h2[r]))):
                        p = pm[li]
                        MM([w, src], [p], p[:], lhsT=w[:], rhs=src[:], start=True, stop=True)
                        A("dve", "tensor_scalar", [p, fb], [arg[r]], out=arg[r][:], in0=p[:], scalar1=fb[:, li:li + 1],
                          scalar2=fb[:, 2 + li:3 + li], op0=ALU.add, op1=ALU.mult)
                        A("act", "activation", [arg[r]], [hh], out=hh[:], in_=arg[r][:], func=AF.Sin, scale=1.0 / 16)
                        A("act", "activation", [arg[r], npi], [cs[r]], out=cs[r][:], in_=arg[r][:], func=AF.Sin, scale=1.0 / 16,
                          bias=npi[:, 0:1])
                        for _dbl in range(4):
                            A("pool", "tensor_tensor", [hh], [s2[r]], out=s2[r][:], in0=hh[:], in1=hh[:], op=ALU.mult)
                            A("dve", "scalar_tensor_tensor", [hh, cs[r]], [hh], out=hh[:], in0=hh[:], scalar=2.0, in1=cs[r][:],
                              op0=ALU.mult, op1=ALU.mult)
                            A("dve", "tensor_scalar", [s2[r]], [cs[r]], out=cs[r][:], in0=s2[r][:], scalar1=-2.0, scalar2=1.0,
                              op0=ALU.mult, op1=ALU.add)
                        src = hh
                    for j in range(4):
                        tix = pb * 4 + j
                        dirsel = 0 if tix < 32 else 1
                        q = (pb * 4 + j) % 2
                        p = po[q]
                        MM([h2[r], w3], [p], p[:], lhsT=h2[r][:, j * 128:(j + 1) * 128], rhs=w3[:, dirsel, :], start=True, stop=True)
                        DMA([dec_in], [dc[q]], out=dc[q][:], in_=dec_in[variant, tix * 128:(tix + 1) * 128, :])
                        A("dve", "tensor_tensor", [p, dc[q]], [hf[q]], out=hf[q][:],
                          in0=p[:].rearrange("p (o c) -> p o c", o=2), in1=dc[q][:].unsqueeze(1).to_broadcast([128, 2, 256]),
                          op=ALU.mult)
                        for o in range(2):
                            DMA([hf[q]], [(HF, (o, tix))], out=HF[o, tix * 128:(tix + 1) * 128, :], in_=hf[q][:, o, :])
            with Scope(B) as st:
                f1b = hy_load_small(st, layer)
                for o in range(2):
                    hy_stage1(HF, HF[o], 128, f1b)

                    def consume(st2, f2, p, state=None, o=o):
                        if f2 is None:
                            return B.ring(st2, "hcb_", [128, 512], BF16, 3)
                        if f2 == 65:
                            return
                        t = state[f2 % 3]
                        B.copy("act" if f2 % 2 else "dve", p, t, out=t[:], in_=p[:])
                        DMA([t], [(HC, (variant, o, f2))], out=HC[variant, o, f2], in_=t[:])
                    hy_stage2((2, 3), consume)

        def hy_conv(layer, variant, row0, nrows, o, src_b, src_ap, dst_b, dst_ap_fn, gcol):
            nt2 = nrows // 64
            with Scope(B) as st:
                f1b = hy_load_small(st, layer)
                hy_stage1(src_b, src_ap, nt2, f1b)

            def consume(st2, f2, p, state=None):
                if f2 is None:
                    me = B.sb(st2, "me", [128, 65, 128], BF16)
                    DMA([MTB], [me], out=me[:], in_=MTB[:, :, 4, :])
                    return {"me": me, "hc": B.ring(st2, "hcl", [128, 512], BF16, 3),
                            "tmp": B.ring(st2, "s2tmp", [128, 512], BF16, 2),
                            "call": B.sb(st2, "Call", [128, 65, 256], BF16),
                            "pc": B.ring(st2, "s2pc", [128, 256], F32, 2, psum=True)}
                if f2 == 65:
                    call = state["call"]
                    for ri in range(2):
                        DMA([call], [(CD, ri)], out=CD[ri].rearrange("f t c -> t f c"), in_=call[ri * 64:(ri + 1) * 64])
                    return
                hc = state["hc"][f2 % 3]
                tmp = state["tmp"][f2 % 2]
                pc = state["pc"][f2 % 2]
                DMA([HC], [hc], out=hc[:], in_=HC[variant, o, f2])
                A("dve", "tensor_tensor", [p, hc], [tmp], out=tmp[:], in0=p[:], in1=hc[:], op=ALU.mult)
                MM([state["me"], tmp], [pc], pc[:], lhsT=state["me"][:, f2, :], rhs=tmp[:, 0:256], start=True, stop=False)
                MM([state["me"], tmp], [pc], pc[:], lhsT=state["me"][:, f2, :], rhs=tmp[:, 256:512], start=False, stop=True)
                B.copy("act", pc, (state["call"], f2), out=state["call"][:, f2, :], in_=pc[:])
            hy_stage2((0, 1), consume)
            with Scope(B) as st:
                gf = B.sb(st, "gfin", [65, 2, 64], F32)
                gb = B.sb(st, "gbin", [65, 2, 64], BF16)
                dd = B.sb(st, "ddb", [128, 512], F32)
                DMA([g_in], [gf], out=gf[:], in_=g_in[:])
                A("pool", "tensor_copy", [gf], [gb], out=gb[:], in_=gf[:])
                DMA([hyd2_in], [dd], out=dd[:], in_=hyd2_in[layer, o].partition_broadcast(128))
                Cr = B.sb(st, "Cr", [65, 16384], BF16)
                Ci = B.sb(st, "Ci", [65, 16384], BF16)
                Ld = B.sb(st, "Ld2", [128, 16384], BF16)
                DMA([CD], [Cr], out=Cr[:], in_=CD[0].rearrange("f t c -> f (t c)"))
                DMA([CD], [Ci], out=Ci[:], in_=CD[1].rearrange("f t c -> f (t c)"))
                DMA([src_b], [Ld], out=Ld[0:nt2, :], in_=src_ap.rearrange("(a b) c -> a (b c)", b=64))
                gt = B.ring(st, "gt", [128, 4096], F32, 2)
                t1_ = B.ring(st, "hy_t1", [128, 512], F32, 2)
                t2_ = B.ring(st, "hy_t2", [128, 512], F32, 2)
                ot = B.ring(st, "hy_ot", [128, 4096], BF16, 2)
                py = B.ring(st, "s3p", [128, 512], F32, 3, psum=True)
                HUv = HU[row0:row0 + nrows, gcol:gcol + 256].rearrange("(a b) c -> a b c", b=64)
                for g in range(4):
                    g_ = gt[g % 2]
                    o_ = ot[g % 2]
                    DMA([HU], [g_], out=g_[0:nt2, :].rearrange("a (b c) -> a b c", c=256), in_=HUv[:, g * 16:(g + 1) * 16, :])
                    for s8 in range(8):
                        s = g * 8 + s8
                        sl = slice(s * 512, (s + 1) * 512)
                        p = py[s % 3]
                        MM([gb, Cr], [p], p[0:nt2, :], lhsT=gb[:, 0, 0:nt2], rhs=Cr[:, sl], start=True, stop=False)
                        MM([gb, Ci], [p], p[0:nt2, :], lhsT=gb[:, 1, 0:nt2], rhs=Ci[:, sl], start=False, stop=True)
                        a_ = t1_[s % 2]
                        b_ = t2_[s % 2]
                        A("pool", "tensor_tensor", [Ld, dd], [a_], out=a_[0:nt2, :], in0=Ld[0:nt2, sl], in1=dd[0:nt2, :], op=ALU.mult)
                        A("dve", "tensor_tensor", [p, a_], [b_], out=b_[0:nt2, :], in0=p[0:nt2, :], in1=a_[0:nt2, :], op=ALU.add)
                        A("pool", "tensor_tensor", [b_, g_], [(o_, s8)], out=o_[0:nt2, s8 * 512:(s8 + 1) * 512], in0=b_[0:nt2, :],
                          in1=g_[0:nt2, s8 * 512:(s8 + 1) * 512], op=ALU.mult)
                    DMA([o_], [dst_b], out=dst_ap_fn(g), in_=o_[0:nt2, :].rearrange("a (b c) -> a b c", c=256))

        def hyena_mixer(layer, last):
            hy_conv_pass(layer)
            seqs = [(0, 256, SEQ)]
            if not last:
                seqs.append((1, 0, CTX))
            for variant, row0, nrows in seqs:
                hy_filters(layer, variant)
                y1v = HY1[row0:row0 + nrows, :].rearrange("(a b) c -> a b c", b=64)
                ymv = YM[row0:row0 + nrows, 0:256].rearrange("(a b) c -> a b c", b=64)
                hy_conv(layer, variant, row0, nrows, 0, HV, HV[row0:row0 + nrows, :], HY1,
                        lambda g, y1v=y1v: y1v[:, g * 16:(g + 1) * 16, :], 256)
                hy_conv(layer, variant, row0, nrows, 1, HY1, HY1[row0:row0 + nrows, :], YM,
                        lambda g, ymv=ymv: ymv[:, g * 16:(g + 1) * 16, :], 512)

        zero_pad_rows()
        hy_cast_tables()
        for layer in range(DEPTH):
            last = layer == DEPTH - 1
            only_gdn = bool(stop_after and (len(stop_after) > 2 or stop_after[0] == "HY"))
            with Scope(B) as st:
              if not only_gdn:
                cc = B.sb(st, "cc", [128, 8, 2], F32)
                scc = B.sb(st, "scc", [128, 8, 2], F32)
                bm = B.sb(st, "bm", [2, 6 * D], F32)
                modrow = B.sb(st, "modrow", [2, 6 * D], F32)
                wst = B.ring(st, "wmst", [128, 8, 512], F32, 2)
                pmod = B.ring(st, "pmod", [2, 512], F32, 2, psum=True)
                DMA([cc_in], [cc], out=cc[:], in_=cc_in[:])
                DMA([bmod_in], [bm], out=bm[:], in_=bmod_in[layer:layer + 1, :].partition_broadcast(2))
                A("act", "activation", [cc], [scc], out=scc[:], in_=cc[:], func=AF.Silu)
                for s in range(12):
                    w = wst[s % 2]
                    pm = pmod[s % 2]
                    DMA([wmod_in], [w], out=w[:], in_=wmod_in[layer, :, :, s * 512:(s + 1) * 512])
                    for k in range(8):
                        MM([scc, w], [pm], pm[:], lhsT=scc[:, k, :], rhs=w[:, k, :], start=(k == 0), stop=(k == 7))
                    A("dve", "tensor_tensor", [pm, bm], [(modrow, s)], out=modrow[:, s * 512:(s + 1) * 512],
                      in0=pm[:], in1=bm[:, s * 512:(s + 1) * 512], op=ALU.add)
                DMA([modrow], [(MOD, layer)], out=MOD[layer], in_=modrow[:])

            def mod_tiles(st, which, segs, norm_g=None):
                res = {}
                for name, seg in segs:
                    t = load_bcast(st, name, (MOD, layer), MOD[layer, which:which + 1, seg * D:(seg + 1) * D])
                    res[name] = t
                return res

            with Scope(B) as st:
              if not only_gdn:
                winb = load_weight_bf16(st, "winb", win_in, win_in[layer], 8, N_IN)
                g1n = load_bcast(st, "g1n", n1g_in, n1g_in[layer:layer + 1, :])
                sh1 = B.sb(st, "sh1", [128, D], F32)
                gs1 = B.sb(st, "gs1", [128, D], F32)
                xr = B.ring(st, "xr", [128, D], F32, 2)
                junk = B.sb(st, "junk", [128, D], BF16)
                rstd = B.ring(st, "rstd", [128, 1], F32, 2)
                tmp = B.sb(st, "tmp", [128, D], F32)
                hb = B.ring(st, "hb", [128, D], BF16, 2)
                hT = B.ring(st, "hT", [128, 8, 128], BF16, 2)
                zt = B.ring(st, "zt", [128, N_IN], F32, 2)
                tp = B.ring(st, "tp", [128, D], BF16, 2, psum=True)
                pz = B.ring(st, "pz", [128, 512], F32, 4, psum=True)
                for ti in range(NT):
                    if ti == 0 or ti == 2:
                        which = 1 if ti == 0 else 0
                        DMA([(MOD, layer)], [sh1], out=sh1[:],
                            in_=MOD[layer, which:which + 1, 0:D].partition_broadcast(128))
                        DMA([(MOD, layer)], [gs1], out=gs1[:],
                            in_=MOD[layer, which:which + 1, D:2 * D].partition_broadcast(128))
                        A("dve", "scalar_tensor_tensor", [gs1, g1n], [gs1], out=gs1[:], in0=gs1[:], scalar=1.0,
                          in1=g1n[:], op0=ALU.add, op1=ALU.mult)
                    r = ti % 2
                    xb_, xap = x_src(layer, ti)
                    DMA([xb_], [xr[r]], out=xr[r][:], in_=xap)
                    rms_rstd(xr[r], junk, rstd[r])
                    A("dve", "scalar_tensor_tensor", [xr[r], rstd[r], gs1], [tmp], out=tmp[:], in0=xr[r][:],
                      scalar=rstd[r][:, 0:1], in1=gs1[:], op0=ALU.mult, op1=ALU.mult)
                    A("pool", "tensor_tensor", [tmp, sh1], [hb[r]], out=hb[r][:], in0=tmp[:], in1=sh1[:], op=ALU.add)
                    transpose_tile(hb[r], lambda k, r=r: hb[r][:, k * 128:(k + 1) * 128], tp[r], hT[r], hT[r][:])
                    for cg in range(7):
                        c0 = cg * 512
                        c1 = min(N_IN, c0 + 512)
                        p = pz[(ti * 7 + cg) % 4]
                        for k in range(8):
                            MM([hT[r], winb], [p], p[:, 0:c1 - c0], lhsT=hT[r][:, k, :], rhs=winb[:, k, c0:c1],
                               start=(k == 0), stop=(k == 7))
                        B.copy(B.evac_eng(), p, (zt[r], cg), out=zt[r][:, c0:c1], in_=p[:, 0:c1 - c0])
                    DMA([zt[r]], [(Z, ti)], out=Z[4 + ti * 128:4 + (ti + 1) * 128, :], in_=zt[r][:])

            if stop_after == ("P1", layer):
                break

            if stop_after and stop_after[0] == "HY":
                hyena_mixer(layer, last)
                break
            if not only_gdn:
                hyena_mixer(layer, last)
                gla_mixer(layer, last)
            gdn_mixer(layer, last)
            if stop_after and (stop_after in (("MIX", layer), ("GDNPREP", layer)) or stop_after[0] == "GDN"):
                break

            tiles5 = range(NT) if not last else range(2, NT)
            with Scope(B) as st:
                woutb = load_weight_bf16(st, "woutb", wout_in, wout_in[layer], 8, D)
                g1t = B.sb(st, "g1t", [128, D], F32)
                xr = B.ring(st, "xr5", [128, D], F32, 2)
                ym = B.ring(st, "ym", [128, D], BF16, 2)
                mT = B.ring(st, "mT", [128, 8, 128], BF16, 2)
                tmp = B.ring(st, "tmp5", [128, 512], F32, 2)
                tp = B.ring(st, "tp5", [128, D], BF16, 2, psum=True)
                py = B.ring(st, "py", [128, 512], F32, 4, psum=True)
                for ti in tiles5:
                    if ti == 0 or ti == 2:
                        which = 1 if ti == 0 else 0
                        DMA([(MOD, layer)], [g1t], out=g1t[:],
                            in_=MOD[layer, which:which + 1, 2 * D:3 * D].partition_broadcast(128))
                    r = ti % 2
                    xb_, xap = x_src(layer, ti)
                    DMA([xb_], [xr[r]], out=xr[r][:], in_=xap)
                    DMA([YM], [ym[r]], out=ym[r][:], in_=YM[ti * 128:(ti + 1) * 128, :])
                    transpose_tile(ym[r], lambda k, r=r: ym[r][:, k * 128:(k + 1) * 128], tp[r], mT[r], mT[r][:])
                    for cg in range(2):
                        p = py[(ti * 2 + cg) % 4]
                        for k in range(8):
                            MM([mT[r], woutb], [p], p[:], lhsT=mT[r][:, k, :], rhs=woutb[:, k, cg * 512:(cg + 1) * 512],
                               start=(k == 0), stop=(k == 7))
                        tt = tmp[cg]
                        A("dve", "tensor_tensor", [p, g1t], [tt], out=tt[:], in0=p[:], in1=g1t[:, cg * 512:(cg + 1) * 512],
                          op=ALU.mult)
                        A("pool", "tensor_tensor", [tt, xr[r]], [xr[r]], out=xr[r][:, cg * 512:(cg + 1) * 512],
                          in0=xr[r][:, cg * 512:(cg + 1) * 512], in1=tt[:], op=ALU.add)
                    DMA([xr[r]], [(XS, ti)], out=XS[ti * 128:(ti + 1) * 128, :], in_=xr[r][:])

            with Scope(B) as st:
                w1b = load_weight_bf16(st, "w1b", w1_in, w1_in[layer], 8, D_FF)
                w2b = load_weight_bf16(st, "w2b", w2_in, w2_in[layer], 32, D)
                g2n = load_bcast(st, "g2n", n2g_in, n2g_in[layer:layer + 1, :])
                sh2 = B.sb(st, "sh2", [128, D], F32)
                gs2 = B.sb(st, "gs2", [128, D], F32)
                g2t = B.sb(st, "g2t", [128, D], F32)
                xr = B.ring(st, "xr6", [128, D], F32, 4)
                junk = B.sb(st, "junk6", [128, D], BF16)
                rstd = B.ring(st, "rstd6", [128, 1], F32, 2)
                tmp = B.sb(st, "tmp6", [128, D], F32)
                hb = B.ring(st, "hb6", [128, D], BF16, 2)
                hT2 = B.ring(st, "hT2", [128, 8, 256], BF16, 2)
                uT = B.sb(st, "uT", [128, 32, 256], BF16)
                rl = B.ring(st, "rl", [128, 256], BF16, 2)
                tt2 = B.ring(st, "tt2", [128, 512], F32, 2)
                tp = B.ring(st, "tp6", [128, D], BF16, 2, psum=True)
                pu = B.ring(st, "pu", [128, 256], F32, 3, psum=True)
                py = B.ring(st, "py6", [128, 512], F32, 2, psum=True)
                groups = [(0, 1)] if not last else []
                groups += [(2 + 2 * g, 3 + 2 * g) for g in range(16)]
                for gi, grp in enumerate(groups):
                    if grp[0] == 0 or grp[0] == 2:
                        which = 1 if grp[0] == 0 else 0
                        for tl, seg in ((sh2, 3), (gs2, 4), (g2t, 5)):
                            DMA([(MOD, layer)], [tl], out=tl[:],
                                in_=MOD[layer, which:which + 1, seg * D:(seg + 1) * D].partition_broadcast(128))
                        A("dve", "scalar_tensor_tensor", [gs2, g2n], [gs2], out=gs2[:], in0=gs2[:], scalar=1.0,
                          in1=g2n[:], op0=ALU.add, op1=ALU.mult)
                    hT = hT2[gi % 2]
                    xs = []
                    for j, ti in enumerate(grp):
                        xt = xr[(gi % 2) * 2 + j]
                        xs.append(xt)
                        DMA([(XS, ti)], [xt], out=xt[:], in_=XS[ti * 128:(ti + 1) * 128, :])
                        rs = rstd[j]
                        rms_rstd(xt, junk, rs)
                        A("dve", "scalar_tensor_tensor", [xt, rs, gs2], [tmp], out=tmp[:], in0=xt[:],
                          scalar=rs[:, 0:1], in1=gs2[:], op0=ALU.mult, op1=ALU.mult)
                        A("pool", "tensor_tensor", [tmp, sh2], [hb[j]], out=hb[j][:], in0=tmp[:], in1=sh2[:], op=ALU.add)
                        transpose_tile(hb[j], lambda k, j=j: hb[j][:, k * 128:(k + 1) * 128], tp[j], (hT, j),
                                       hT[:, :, j * 128:(j + 1) * 128])
                    for m in range(32):
                        p = pu[m % 3]
                        for k in range(8):
                            MM([hT, w1b], [p], p[:], lhsT=w1b[:, k, m * 128:(m + 1) * 128], rhs=hT[:, k, :],
                               start=(k == 0), stop=(k == 7))
                        rr_ = rl[m % 2]
                        A("act", "activation", [p], [rr_], out=rr_[:], in_=p[:], func=AF.Relu)
                        A("pool" if m % 2 else "dve", "tensor_tensor", [rr_], [(uT, m)], out=uT[:, m, :], in0=rr_[:],
                          in1=rr_[:], op=ALU.mult)
                    for j, ti in enumerate(grp):
                        xt = xs[j]
                        for cg in range(2):
                            p = py[cg]
                            for m in range(32):
                                MM([uT, w2b], [p], p[:], lhsT=uT[:, m, j * 128:(j + 1) * 128],
                                   rhs=w2b[:, m, cg * 512:(cg + 1) * 512], start=(m == 0), stop=(m == 31))
                            tt = tt2[cg]
                            A("dve", "tensor_tensor", [p, g2t], [tt], out=tt[:], in0=p[:],
                              in1=g2t[:, cg * 512:(cg + 1) * 512], op=ALU.mult)
                            A("pool", "tensor_tensor", [tt, xt], [xt], out=xt[:, cg * 512:(cg + 1) * 512],
                              in0=xt[:, cg * 512:(cg + 1) * 512], in1=tt[:], op=ALU.add)
                        DMA([xt], [(XS, ti)], out=XS[ti * 128:(ti + 1) * 128, :], in_=xt[:])

        with Scope(B) as st:
            fg = load_bcast(st, "fg", fng_in, fng_in[0:1, :])
            xr = B.ring(st, "xrf", [128, D], F32, 2)
            orr = B.ring(st, "orr", [128, D], F32, 2)
            junk = B.sb(st, "junkf", [128, D], BF16)
            rstd = B.ring(st, "rstdf", [128, 1], F32, 2)
            for ti in range(2, NT):
                r = ti % 2
                DMA([(XS, ti)], [xr[r]], out=xr[r][:], in_=XS[ti * 128:(ti + 1) * 128, :])
                rms_rstd(xr[r], junk, rstd[r])
                A("dve", "scalar_tensor_tensor", [xr[r], rstd[r], fg], [orr[r]], out=orr[r][:], in0=xr[r][:],
                  scalar=rstd[r][:, 0:1], in1=fg[:], op0=ALU.mult, op1=ALU.mult)
                DMA([orr[r]], [(out_d, ti)], out=out_d[(ti - 2) * 128:(ti - 1) * 128, :], in_=orr[r][:])

        P._emit_waits("sp", [t for t in P.dma_last_tok if t is not None])
        P.emit(top)
    return nc


def mix_stub(B, layer, YM):
    with Scope(B) as st:
        z = B.sb(st, "zero", [128, D], BF16)
        B.A("pool", "memset", [], [z], ap=z[:], constant=0.0)
        for ti in range(NT):
            B.DMA([z], [(YM, ("hy", ti))], out=YM[ti * 128:(ti + 1) * 128, 0:256], in_=z[:, 0:256])


def _kchunk(w, kch):
    d, r, n = w.shape
    return np.ascontiguousarray(w.reshape(d, kch, 128, n).transpose(0, 2, 1, 3))


def _gdn_consts():
    ii = np.arange(128)
    p, q = ii[:, None], ii[None, :]
    one = lambda c: c.astype(np.float32)
    tri1 = np.stack([one(p <= q), one(p >= q), one(p > q), one(p < q)], axis=1)
    neg = lambda c: np.where(c, 0.0, -30000.0).astype(np.float32)
    nmask = np.stack([neg(p <= q), neg(p >= q), neg(q < p), neg(q > p)], axis=1)
    esel = np.zeros((8, 8, 128), np.float32)
    for k in range(8):
        esel[k, k, :] = 1.0
    cm = np.zeros((128, 3, 7), np.float32)
    for s in range(-3, 4):
        cm[:, 0, s + 3] = ((ii % 64 + s >= 0) & (ii % 64 + s < 64))
        cm[:, 1, s + 3] = (ii + s >= 0)
        cm[:, 2, s + 3] = (128 + ii + s < 256)
    gm = np.zeros((128, 7, 128), np.float32)
    for lv in range(6):
        sz = 2 ** (lv + 1)
        gm[:, lv, :] = ((p // (2 * sz) == q // (2 * sz)) & (p // sz != q // sz))
    gm[:, 6, :] = (p // 2 == q // 2)
    return {"tri1": np.ascontiguousarray(tri1), "nmask": np.ascontiguousarray(nmask), "esel": esel, "cmask": cm, "gmask": gm}


def _hy_consts():
    N = 8192
    t2 = np.arange(128)[:, None]
    f2 = np.arange(65)[None, :]
    ang = 2 * np.pi * t2 * f2 / 128.0
    F1 = np.stack([np.cos(ang), -np.sin(ang)], axis=1)
    t1 = np.arange(64)[:, None]
    f1 = np.arange(64)[None, :]
    MT = np.zeros((65, 128, 5, 128))
    blk = lambda a, b, c, d: np.block([[a, b], [c, d]])
    for k in range(65):
        ph = -2 * np.pi * (t1 * f1 / 64.0 + t1 * k / 8192.0)
        Mr, Mi = np.cos(ph), np.sin(ph)
        MT[k, :, 0] = blk(Mr, Mi, -Mi, Mr)
        MT[k, :, 1] = blk(Mi, Mr, Mr, -Mi)
        MT[k, :, 2] = blk(Mr, Mr, -Mi, -Mi)
        MT[k, :, 3] = blk(-Mi, Mi, -Mr, Mr)
        MT[k, :, 4] = blk(Mr.T, -Mi.T, Mi.T, Mr.T)
    a = np.full(65, 2.0)
    a[0] = 1.0
    a[64] = 1.0
    ang2 = 2 * np.pi * np.arange(64)[None, :] * np.arange(65)[:, None] / 128.0
    G = np.stack([a[:, None] * np.cos(ang2) / N, -a[:, None] * np.sin(ang2) / N], axis=1)
    zpos = np.zeros((2, 33, N), np.float32)
    dec = np.zeros((2, N, 256), np.float32)
    deltas = np.abs(np.linspace(math.log(1e-2) / 1.5, math.log(1e-2) / 0.3, 256, dtype=np.float32))
    fq = np.linspace(1e-4, 15, 16, dtype=np.float32)
    for vi, L in enumerate((4096, 256)):
        t = np.linspace(0.0, 1.0, L, dtype=np.float32)
        w = (2 * math.pi * np.arange(L, dtype=np.float32) / L).astype(np.float32)
        angp = w[:, None] * fq[None, :]
        z = np.concatenate([t[:, None], np.cos(angp), -np.sin(angp)], axis=-1).astype(np.float32)
        dw = np.exp(-t[:, None] * deltas[None, :]).astype(np.float32)
        zpos[vi, :, :L] = z.T
        dec[vi, :L] = dw
        pidx = np.arange(1, L)
        zpos[vi][:, N - pidx] = z[pidx].T
        dec[vi][N - pidx] = dw[pidx]
    return {"hy_F1": F1.astype(np.float32), "hy_MT": np.ascontiguousarray(MT.transpose(1, 0, 2, 3)).astype(np.float32),
            "hy_G": G.astype(np.float32), "hy_zpos": zpos, "hy_dec": dec}


_NC_CACHE = {}


def kernel(x, c, ctx, c_ctx, norm1_g, norm2_g, w_mod, b_mod, w_in, w_out, hy_conv_w, hy_conv_b,
           hy_f_w1, hy_f_b1, hy_f_w2, hy_f_b2, hy_f_w3, hy_sin_freq, hy_d, gla_w_a2, gla_b_a, gla_norm_g,
           gdn_conv_w, gdn_a_log, gdn_dt_bias, gdn_norm_g, w_mlp1, w_mlp2, final_norm_g):
    f = lambda a: np.ascontiguousarray(np.asarray(a, dtype=np.float32))
    x, c, ctx, c_ctx = f(x), f(c), f(ctx), f(c_ctx)
    shared = {
        "norm1_g": f(norm1_g), "norm2_g": f(norm2_g),
        "w_mod": _kchunk(f(w_mod), 8), "b_mod": f(b_mod),
        "w_in": _kchunk(f(w_in), 8), "w_out": _kchunk(f(w_out), 8),
        "w_mlp1": _kchunk(f(w_mlp1), 8), "w_mlp2": _kchunk(f(w_mlp2), 32),
        "final_norm_g": f(final_norm_g).reshape(1, D),
    }
    ii = np.arange(128)
    jle = (ii[:, None] <= ii[None, :]).astype(np.float32)
    jge = (ii[:, None] >= ii[None, :]).astype(np.float32)
    mgt = (ii[:, None] > ii[None, :]).astype(np.float32)
    mlt = (ii[:, None] < ii[None, :]).astype(np.float32)
    shared["tri"] = np.ascontiguousarray(np.stack([jle, jge, mgt, mlt], axis=1) * np.float32(-1.0 / 16.0))
    shared["mask4"] = np.ascontiguousarray(np.stack([np.tile(jle, (1, 4)), np.tile(jge, (1, 4))], axis=1))
    wa = np.zeros((DEPTH, 33, 384), np.float32)
    w_a2, b_a = f(gla_w_a2), f(gla_b_a)
    wa[:, 0:16, 0:192] = w_a2[:, 0]
    wa[:, 16:32, 192:384] = w_a2[:, 1]
    wa[:, 32, 0:192] = b_a[:, 0]
    wa[:, 32, 192:384] = b_a[:, 1]
    shared["gla_wa"] = wa
    shared["gla_ng"] = f(gla_norm_g)
    shared.update(_gdn_consts())
    shared["gdn_cw"] = f(gdn_conv_w).reshape(DEPTH, 1, 7 * 1152)
    shared["gdn_par"] = np.concatenate([f(gdn_a_log).reshape(DEPTH, 8), f(gdn_dt_bias).reshape(DEPTH, 8)],
                                       axis=1).reshape(DEPTH, 1, 16)
    shared["gdn_ng"] = f(gdn_norm_g)
    shared.update(_hy_consts())
    shared["hy_cw"] = f(hy_conv_w).reshape(DEPTH, 1, 3 * 768)
    shared["hy_cb"] = f(hy_conv_b).reshape(DEPTH, 1, 768)
    shared["hy_w1"] = f(hy_f_w1)
    shared["hy_w2"] = f(hy_f_w2)
    shared["hy_w3r"] = np.ascontiguousarray(f(hy_f_w3).reshape(DEPTH, 64, 2, 2, 256).transpose(0, 1, 3, 2, 4)).reshape(DEPTH, 64, 2, 512)
    sf = f(hy_sin_freq)
    shared["hy_fb"] = np.ascontiguousarray(np.stack([f(hy_f_b1), f(hy_f_b2), sf[:, 0], sf[:, 1]], axis=-1))
    shared["hy_d2"] = np.ascontiguousarray(np.tile(f(hy_d), (1, 1, 2)).reshape(DEPTH, 2, 1, 512))
    in_maps = []
    for b in range(8):
        cc = np.stack([c[b], c_ctx], axis=-1).reshape(8, 128, 2).transpose(1, 0, 2)
        m = dict(shared)
        m["x"] = x[b]
        m["ctx"] = ctx[b]
        m["cc"] = np.ascontiguousarray(cc)
        in_maps.append(m)
    if "nc" not in _NC_CACHE:
        _NC_CACHE["nc"] = build_program()
    nc = _NC_CACHE["nc"]
    res = run_bass_kernel_spmd(nc, in_maps, core_ids=list(range(8)))
    return np.stack([np.asarray(r["out"], dtype=np.float32) for r in res.results], axis=0)
```

```python
from contextlib import ExitStack
import math
import numpy as np
import concourse.bass as bass
import concourse.mybir as mybir
from concourse.bass_utils import run_bass_kernel_spmd

F32 = mybir.dt.float32
BF16 = mybir.dt.bfloat16
AF = mybir.ActivationFunctionType
ALU = mybir.AluOpType

D = 1024
SEQ = 4096
CTX = 256
T = SEQ + CTX
NT = T // 128
DEPTH = 2
N_IN = 3504
D_FF = 4096
EPS = 1e-6
HY0 = 0
GLA0 = 768
GDN0 = 768 + 1184

ENGS = ("pe", "act", "dve", "pool", "sp")
N_DMA_SEMS = 24


class Buf:
    __slots__ = ("name", "t", "whole_w", "whole_r", "sub")

    def __init__(self, name, t):
        self.name = name
        self.t = t
        self.whole_w = None
        self.whole_r = []
        self.sub = {}

    def __getitem__(self, idx):
        return self.t[idx]


def _compress(toks):
    d = {}
    for k, v in toks:
        if d.get(k, 0) < v:
            d[k] = v
    return list(d.items())


class Prog:
    def __init__(self, nc):
        self.nc = nc
        self.ops = {e: [] for e in ENGS}
        self.cnt = {e: 0 for e in ENGS}
        self.known = {e: {} for e in ENGS}
        self.snaps = {e: {} for e in ENGS}
        self.dma_cnt = [0] * N_DMA_SEMS
        self.dma_next = 0
        self.dma_last_tok = [None] * N_DMA_SEMS
        self.dma_snaps = {}
        self.n_wait = 0
        self.n_ins = 0

    def _deps(self, reads, writes):
        deps = []
        for key in reads:
            b, k = key if isinstance(key, tuple) else (key, None)
            if b.whole_w is not None:
                deps.append(b.whole_w)
            if k is None:
                for (w, r) in b.sub.values():
                    if w is not None:
                        deps.append(w)
            else:
                s = b.sub.get(k)
                if s is not None and s[0] is not None:
                    deps.append(s[0])
        for key in writes:
            b, k = key if isinstance(key, tuple) else (key, None)
            if b.whole_w is not None:
                deps.append(b.whole_w)
            deps.extend(b.whole_r)
            if k is None:
                for (w, r) in b.sub.values():
                    if w is not None:
                        deps.append(w)
                    deps.extend(r)
            else:
                s = b.sub.get(k)
                if s is not None:
                    if s[0] is not None:
                        deps.append(s[0])
                    deps.extend(s[1])
        return deps

    def _commit(self, tok, reads, writes):
        for key in reads:
            b, k = key if isinstance(key, tuple) else (key, None)
            if k is None:
                b.whole_r.append(tok)
                if len(b.whole_r) > 40:
                    b.whole_r = _compress(b.whole_r)
            else:
                s = b.sub.setdefault(k, [None, []])
                s[1].append(tok)
                if len(s[1]) > 40:
                    s[1] = _compress(s[1])
        for key in writes:
            b, k = key if isinstance(key, tuple) else (key, None)
            if k is None:
                b.whole_w = tok
                b.whole_r = []
                b.sub = {}
            else:
                b.sub[k] = [tok, []]

    def _emit_waits(self, eng, deps):
        kn = self.known[eng]
        need = {}
        for tok in deps:
            if tok is None:
                continue
            semkey, val = tok
            if semkey == eng and eng == "pe":
                continue
            if kn.get(semkey, 0) >= val:
                continue
            if need.get(semkey, 0) < val:
                need[semkey] = val
        for semkey, val in need.items():
            if kn.get(semkey, 0) >= val:
                continue
            self.ops[eng].append(("wait", semkey, val))
            self.n_wait += 1
            kn[semkey] = val
            if isinstance(semkey, str):
                snap = self.snaps[semkey].get(val)
            else:
                snap = self.dma_snaps.get((semkey, val))
            if snap:
                for k2, v2 in snap.items():
                    if kn.get(k2, 0) < v2:
                        kn[k2] = v2

    def op(self, eng, fn, reads=(), writes=(), inc=True):
        deps = self._deps(reads, writes)
        self._emit_waits(eng, deps)
        self.cnt[eng] += 1
        tok = (eng, self.cnt[eng])
        self.ops[eng].append(("ins", fn, self.cnt[eng]))
        self.snaps[eng][self.cnt[eng]] = dict(self.known[eng])
        self.n_ins += 1
        self._commit(tok, reads, writes)
        return tok

    def dma(self, fn, reads=(), writes=(), eng="sp"):
        deps = self._deps(reads, writes)
        s = self.dma_next
        self.dma_next = (self.dma_next + 1) % N_DMA_SEMS
        semkey = ("dma", s)
        if self.dma_last_tok[s] is not None:
            deps.append(self.dma_last_tok[s])
        self._emit_waits(eng, deps)
        self.dma_cnt[s] += 16
        tok = (semkey, self.dma_cnt[s])
        self.dma_last_tok[s] = tok
        self.ops[eng].append(("dma", fn, s))
        self.dma_snaps[(semkey, self.dma_cnt[s])] = dict(self.known[eng])
        self.n_ins += 1
        self._commit(tok, reads, writes)
        return tok

    def emit(self, stack):
        nc = self.nc
        sems = {e: stack.enter_context(nc.semaphore("s_" + e)) for e in ENGS}
        dsems = [stack.enter_context(nc.semaphore("d_%d" % i)) for i in range(N_DMA_SEMS)]
        waited = {e: set() for e in ENGS}
        for e in ENGS:
            for item in self.ops[e]:
                if item[0] == "wait" and isinstance(item[1], str):
                    waited[item[1]].add(item[2])
        rank = {e: {} for e in ENGS}
        for e in ENGS:
            c = 0
            for item in self.ops[e]:
                if item[0] == "ins" and item[2] in waited[e]:
                    c += 1
                    rank[e][item[2]] = c
        self.n_inc = {e: len(rank[e]) for e in ENGS}

        block = stack.enter_context(nc.Block())

        def run(e):
            def body(engine):
                for item in self.ops[e]:
                    if item[0] == "wait":
                        if isinstance(item[1], str):
                            engine.wait_ge(sems[item[1]], rank[item[1]][item[2]])
                        else:
                            engine.wait_ge(dsems[item[1][1]], item[2])
                    elif item[0] == "ins":
                        ins = item[1](engine)
                        if item[2] in rank[e]:
                            ins.then_inc(sems[e], 1)
                    else:
                        ins = item[1](engine)
                        ins.then_inc(dsems[item[2]], 16)
            return body

        block.tensor(run("pe"))
        block.scalar(run("act"))
        block.vector(run("dve"))
        block.gpsimd(run("pool"))
        block.sync(run("sp"))


class Scope(ExitStack):
    def __init__(self, builder):
        super().__init__()
        self.builder = builder
        self.bufs = []

    def __exit__(self, *a):
        rel = self.builder.released
        for b in self.bufs:
            toks = [b.whole_w] + list(b.whole_r)
            for (w, r) in b.sub.values():
                toks.append(w)
                toks.extend(r)
            for t in toks:
                if t is not None and rel.get(t[0], 0) < t[1]:
                    rel[t[0]] = t[1]
        return super().__exit__(*a)


class Builder:
    def __init__(self, nc, debug=False):
        self.nc = nc
        self.P = Prog(nc)
        self.debug = debug
        self.uid = 0
        self.rr = 0
        self.released = {}

    def _new(self, st, name, t):
        b = Buf(name, t)
        b.whole_r = list(self.released.items())
        st.bufs.append(b)
        return b

    def sb(self, st, name, shape, dt):
        self.uid += 1
        return self._new(st, name, st.enter_context(self.nc.sbuf_tensor("%s_%d" % (name, self.uid), shape, dt)))

    def ps(self, st, name, shape, dt):
        self.uid += 1
        return self._new(st, name, st.enter_context(self.nc.psum_tensor("%s_%d" % (name, self.uid), shape, dt)))

    def ring(self, st, name, shape, dt, n, psum=False):
        f = self.ps if psum else self.sb
        return [f(st, "%s%d" % (name, i), shape, dt) for i in range(n)]

    def dram(self, name, shape, dt, kind="Internal"):
        return Buf(name, self.nc.dram_tensor(name, shape, dt, kind=kind).ap())

    def A(self, eng, method, reads, writes, **kw):
        return self.P.op(eng, lambda e: getattr(e, method)(**kw), reads, writes)

    def DMA(self, reads, writes, out, in_, eng="sp"):
        return self.P.dma(lambda e: e.dma_start(out=out, in_=in_), reads, writes, eng=eng)

    def MM(self, reads, writes, out, lhsT, rhs, start, stop):
        return self.P.op("pe", lambda e: e.matmul(out, lhsT=lhsT, rhs=rhs, start=start, stop=stop),
                         reads, writes, inc=stop)

    def TR(self, reads, writes, out, in_, ident, inc=True):
        return self.P.op("pe", lambda e: e.transpose(out=out, in_=in_, identity=ident), reads, writes, inc=inc)

    def evac_eng(self):
        self.rr += 1
        return "act" if self.rr % 2 else "dve"

    def copy(self, eng, src_b, dst_b, out, in_):
        if eng == "act":
            return self.A("act", "activation", [src_b], [dst_b], out=out, in_=in_, func=AF.Copy)
        return self.A(eng, "tensor_copy", [src_b], [dst_b], out=out, in_=in_)


def build_program(debug=False, stop_after=None):
    nc = bass.Bass("TRN2", target_bir_lowering=False)
    B = Builder(nc, debug)
    P = B.P
    A, DMA, MM, TR = B.A, B.DMA, B.MM, B.TR

    def inp(name, shape):
        return B.dram(name, shape, F32, kind="ExternalInput")

    x_in = inp("x", [SEQ, D])
    ctx_in = inp("ctx", [CTX, D])
    cc_in = inp("cc", [128, 8, 2])
    n1g_in = inp("norm1_g", [DEPTH, D])
    n2g_in = inp("norm2_g", [DEPTH, D])
    wmod_in = inp("w_mod", [DEPTH, 128, 8, 6 * D])
    bmod_in = inp("b_mod", [DEPTH, 6 * D])
    win_in = inp("w_in", [DEPTH, 128, 8, N_IN])
    wout_in = inp("w_out", [DEPTH, 128, 8, D])
    w1_in = inp("w_mlp1", [DEPTH, 128, 8, D_FF])
    w2_in = inp("w_mlp2", [DEPTH, 128, 32, D])
    fng_in = inp("final_norm_g", [1, D])
    tri_in = inp("tri", [128, 4, 128])
    mask_in = inp("mask4", [128, 2, 512])
    glawa_in = inp("gla_wa", [DEPTH, 33, 384])
    glang_in = inp("gla_ng", [DEPTH, 96])
    tri1_in = inp("tri1", [128, 4, 128])
    nmask_in = inp("nmask", [128, 4, 128])
    esel_in = inp("esel", [8, 8, 128])
    gmask_in = inp("gmask", [128, 7, 128])
    cmask_in = inp("cmask", [128, 3, 7])
    gdncw_in = inp("gdn_cw", [DEPTH, 1, 7 * 1152])
    gdnpar_in = inp("gdn_par", [DEPTH, 1, 16])
    gdnng_in = inp("gdn_ng", [DEPTH, 96])
    hycw_in = inp("hy_cw", [DEPTH, 1, 3 * 768])
    hycb_in = inp("hy_cb", [DEPTH, 1, 768])
    hyw1_in = inp("hy_w1", [DEPTH, 33, 64])
    hyw2_in = inp("hy_w2", [DEPTH, 64, 64])
    hyw3_in = inp("hy_w3r", [DEPTH, 64, 2, 512])
    hyfb_in = inp("hy_fb", [DEPTH, 64, 4])
    hyd2_in = inp("hy_d2", [DEPTH, 2, 1, 512])
    zpos_in = inp("hy_zpos", [2, 33, 8192])
    dec_in = inp("hy_dec", [2, 8192, 256])
    f1_in = inp("hy_F1", [128, 2, 65])
    mt_in = inp("hy_MT", [128, 65, 5, 128])
    g_in = inp("hy_G", [65, 2, 64])
    out_d = B.dram("out", [SEQ, D], F32, kind="ExternalOutput")

    SK = "ExternalOutput" if debug else "Internal"
    XS = B.dram("XS", [T, D], F32, kind=SK)
    Z = B.dram("Z", [T + 8, N_IN], F32, kind=SK)
    YM = B.dram("YM", [T, D], BF16, kind=SK)
    MOD = B.dram("MOD", [DEPTH, 2, 6 * D], F32, kind=SK)
    GQKV = B.dram("GQKV", [T, 1152], BF16, kind=SK)
    GPAR = B.dram("GPAR", [T, 24], F32, kind=SK)
    HU = B.dram("HU", [T, 768], F32, kind=SK)
    HV = B.dram("HV", [T, 256], BF16, kind=SK)
    HY1 = B.dram("HY1", [T, 256], BF16, kind=SK)
    HF = B.dram("HF", [2, 8192, 256], BF16, kind=SK)
    HC = B.dram("HC", [2, 2, 65, 128, 512], BF16, kind=SK)
    BD = B.dram("BD", [2, 65, 64, 256], BF16, kind=SK)
    CD = B.dram("CD", [2, 65, 64, 256], BF16, kind=SK)
    MTB = B.dram("MTB", [128, 65, 5, 128], BF16, kind=SK)
    dbg = {}

    with Scope(B) as top:
        ident = B.sb(top, "ident", [128, 128], BF16)
        eps_t = B.sb(top, "eps", [128, 1], F32)
        A("pool", "memset", [], [ident], ap=ident[:], constant=1.0)
        A("pool", "affine_select", [ident], [ident], out=ident[:], in_=ident[:], pattern=[[-1, 128]],
          compare_op=ALU.is_equal, fill=0.0, base=0, channel_multiplier=1)
        A("pool", "memset", [], [eps_t], ap=eps_t[:], constant=EPS)

        def x_src(layer, ti):
            if layer == 0:
                if ti < 2:
                    return ctx_in, ctx_in[ti * 128:(ti + 1) * 128, :]
                return x_in, x_in[(ti - 2) * 128:(ti - 1) * 128, :]
            return (XS, ti), XS[ti * 128:(ti + 1) * 128, :]

        def load_weight_bf16(st, name, src_b, src_ap3, kch, ncols, slab=256):
            wt = B.sb(st, name, [128, kch, ncols], BF16)
            with Scope(B) as st2:
                stg = B.ring(st2, name + "stg", [128, kch, slab], F32, 2)
                i = 0
                for c0 in range(0, ncols, slab):
                    c1 = min(ncols, c0 + slab)
                    s = stg[i % 2]
                    DMA([src_b], [s], out=s[:, :, 0:c1 - c0], in_=src_ap3[:, :, c0:c1])
                    eng = ("pool", "dve", "act")[i % 3]
                    B.copy(eng, s, (wt, i), out=wt[:, :, c0:c1], in_=s[:, :, 0:c1 - c0])
                    i += 1
            return wt

        def load_bcast(st, name, src_b, row_ap, n=D):
            t = B.sb(st, name, [128, n], F32)
            DMA([src_b], [t], out=t[:], in_=row_ap.partition_broadcast(128))
            return t

        def rms_rstd(xt, junk, rstd):
            A("act", "activation", [xt], [junk, rstd], out=junk[:], in_=xt[:], func=AF.Square, accum_out=rstd[:])
            A("act", "activation", [rstd, eps_t], [rstd], out=rstd[:], in_=rstd[:], func=AF.Sqrt,
              scale=1.0 / D, bias=eps_t[:, 0:1])
            A("dve", "reciprocal", [rstd], [rstd], out=rstd[:], in_=rstd[:])

        def transpose_tile(src, srcap_fn, tp, dst, dstap):
            for k in range(8):
                TR([src, ident], [tp], out=tp[:, k * 128:(k + 1) * 128], in_=srcap_fn(k), ident=ident[:],
                   inc=(k == 7))
            B.copy("act", tp, dst, out=dstap, in_=tp[:].rearrange("p (k t) -> p k t", k=8))


        identf = B.sb(top, "identf", [128, 128], F32)
        A("pool", "memset", [], [identf], ap=identf[:], constant=1.0)
        A("pool", "affine_select", [identf], [identf], out=identf[:], in_=identf[:], pattern=[[-1, 128]],
          compare_op=ALU.is_equal, fill=0.0, base=0, channel_multiplier=1)

        def gla_mixer(layer, last):
            with Scope(B) as st:
                tri = B.sb(st, "tri", [128, 4, 128], F32)
                msk = B.sb(st, "msk", [128, 2, 512], F32)
                wa = B.sb(st, "wa", [33, 384], F32)
                gng = B.sb(st, "gng", [128, 96], F32)
                DMA([tri_in], [tri], out=tri[:], in_=tri_in[:])
                DMA([mask_in], [msk], out=msk[:], in_=mask_in[:])
                DMA([glawa_in], [wa], out=wa[:], in_=glawa_in[layer])
                DMA([glang_in], [gng], out=gng[:], in_=glang_in[layer:layer + 1, :].partition_broadcast(128))
                O = B.sb(st, "glaO", [128, NT, 384], F32)
                S = [B.sb(st, "glaS%d" % d, [48, 384], F32) for d in range(2)]
                Sb = [B.sb(st, "glaSb%d" % d, [48, 384], BF16) for d in range(2)]
                zg = B.ring(st, "zg", [128, 1184], F32, 2)
                aT = B.ring(st, "aT", [33, 128], F32, 2)
                e1 = B.ring(st, "e1", [128, 192], F32, 2)
                sp = B.ring(st, "sp", [128, 192], F32, 2)
                ebT = B.ring(st, "ebT", [48, 512], F32, 2)
                enbT = B.ring(st, "enbT", [48, 512], F32, 2)
                ecs = B.ring(st, "ecs", [128, 192], F32, 2)
                qkb = B.ring(st, "qkb", [128, 384], BF16, 2)
                qtT = B.ring(st, "qtT", [48, 512], BF16, 2)
                ktT = B.ring(st, "ktT", [48, 512], BF16, 2)
                kp = B.ring(st, "kp", [128, 192], BF16, 2)
                vb = B.ring(st, "vb", [128, 384], BF16, 2)
                atT = B.ring(st, "atT", [128, 512], BF16, 2)
                sg = B.ring(st, "sg", [128, 384], F32, 2)
                ssq = B.ring(st, "ssq", [128, 4], F32, 2)
                yn = B.ring(st, "yn", [128, 384], F32, 2)
                yo = B.ring(st, "yo", [128, 384], BF16, 2)
                junk = B.sb(st, "gjunk", [128, 96], F32)
                p_aT = B.ps(st, "p_aT", [32, 128], F32)
                p_gl = B.ps(st, "p_gl", [128, 192], F32)
                p_bT = B.ps(st, "p_bT", [48, 512], F32)
                p_cs = B.ps(st, "p_cs", [128, 192], F32)
                p_qk = B.ps(st, "p_qk", [48, 1024], BF16)
                p_at = B.ps(st, "p_at", [128, 512], F32)
                p_o = B.ps(st, "p_o", [128, 384], F32)
                p_S = B.ps(st, "p_S", [48, 384], F32)
                for r in range(2):
                    A("pool", "memset", [], [aT[r]], ap=aT[r][:], constant=1.0)
                for d in range(2):
                    A("pool", "memset", [], [S[d]], ap=S[d][:], constant=0.0)
                    A("pool", "memset", [], [Sb[d]], ap=Sb[d][:], constant=0.0)
                    order = list(range(NT)) if d == 0 else [1, 0] + list(range(NT - 1, 1, -1))
                    def chunk_gen(it, ti, d=d):
                        r = it % 2
                        z = zg[r]
                        DMA([(Z, ti)], [z], out=z[:], in_=Z[4 + ti * 128:4 + (ti + 1) * 128, GLA0:GLA0 + 1184])
                        TR([z, identf], [p_aT], out=p_aT[:], in_=z[:, 1152:1184], ident=identf[:])
                        B.copy("act", p_aT, aT[r], out=aT[r][0:32, :], in_=p_aT[:])
                        MM([aT[r], wa], [p_gl], p_gl[:], lhsT=aT[r][:], rhs=wa[:, d * 192:(d + 1) * 192], start=True, stop=True)
                        A("act", "activation", [p_gl], [e1[r]], out=e1[r][:], in_=p_gl[:], func=AF.Exp, scale=-1.0)
                        A("act", "activation", [e1[r]], [sp[r]], out=sp[r][:], in_=e1[r][:], func=AF.Ln, bias=1.0)
                        for h in range(4):
                            MM([sp[r], tri], [p_bT], p_bT[:, h * 128:(h + 1) * 128], lhsT=sp[r][:, h * 48:(h + 1) * 48],
                               rhs=tri[:, d, :], start=True, stop=True)
                        MM([sp[r], tri], [p_cs], p_cs[:], lhsT=tri[:, 2 + d, :], rhs=sp[r][:], start=True, stop=True)
                        A("act", "activation", [p_bT], [ebT[r]], out=ebT[r][:], in_=p_bT[:], func=AF.Exp)
                        A("act", "activation", [p_bT], [enbT[r]], out=enbT[r][:], in_=p_bT[:], func=AF.Exp, scale=-1.0)
                        A("act", "activation", [p_cs], [ecs[r]], out=ecs[r][:], in_=p_cs[:], func=AF.Exp)
                        A("act", "activation", [z], [qkb[r]], out=qkb[r][:, 0:192], in_=z[:, 0:192], func=AF.Copy,
                          scale=48.0 ** -0.5)
                        A("pool", "tensor_copy", [z], [qkb[r]], out=qkb[r][:, 192:384], in_=z[:, 192:384])
                        for j in range(8):
                            TR([qkb[r], ident], [p_qk], out=p_qk[:, j * 128:(j + 1) * 128],
                               in_=qkb[r][:, j * 48:(j + 1) * 48], ident=ident[:], inc=(j == 7))
                        A("dve", "tensor_tensor", [p_qk, ebT[r]], [qtT[r]], out=qtT[r][:], in0=p_qk[:, 0:512],
                          in1=ebT[r][:], op=ALU.mult)
                        A("dve", "tensor_tensor", [p_qk, enbT[r]], [ktT[r]], out=ktT[r][:], in0=p_qk[:, 512:1024],
                          in1=enbT[r][:], op=ALU.mult)
                        A("pool", "tensor_tensor", [z, ecs[r]], [kp[r]], out=kp[r][:], in0=z[:, 192:384], in1=ecs[r][:],
                          op=ALU.mult)
                        A("pool", "tensor_copy", [z], [vb[r]], out=vb[r][:], in_=z[:, 384:768])
                        for h in range(4):
                            MM([ktT[r], qtT[r]], [p_at], p_at[:, h * 128:(h + 1) * 128], lhsT=ktT[r][:, h * 128:(h + 1) * 128],
                               rhs=qtT[r][:, h * 128:(h + 1) * 128], start=True, stop=True)
                        A("dve", "tensor_tensor", [p_at, msk], [atT[r]], out=atT[r][:], in0=p_at[:], in1=msk[:, d, :],
                          op=ALU.mult)
                        yield
                        for h in range(4):
                            MM([qtT[r], Sb[d]], [p_o], p_o[:, h * 96:(h + 1) * 96], lhsT=qtT[r][:, h * 128:(h + 1) * 128],
                               rhs=Sb[d][:, h * 96:(h + 1) * 96], start=True, stop=False)
                            MM([atT[r], vb[r]], [p_o], p_o[:, h * 96:(h + 1) * 96], lhsT=atT[r][:, h * 128:(h + 1) * 128],
                               rhs=vb[r][:, h * 96:(h + 1) * 96], start=False, stop=True)
                        if d == 0:
                            B.copy("act", p_o, (O, ti), out=O[:, ti, :], in_=p_o[:])
                        else:
                            A("dve", "tensor_tensor", [p_o, (O, ti)], [(O, ti)], out=O[:, ti, :], in0=p_o[:],
                              in1=O[:, ti, :], op=ALU.add)
                        for h in range(4):
                            MM([kp[r], vb[r]], [p_S], p_S[:, h * 96:(h + 1) * 96], lhsT=kp[r][:, h * 48:(h + 1) * 48],
                               rhs=vb[r][:, h * 96:(h + 1) * 96], start=True, stop=True)
                        col = 127 if d == 0 else 0
                        for h in range(4):
                            A("dve", "scalar_tensor_tensor", [S[d], ebT[r], p_S], [S[d]], out=S[d][:, h * 96:(h + 1) * 96],
                              in0=S[d][:, h * 96:(h + 1) * 96], scalar=ebT[r][:, h * 128 + col:h * 128 + col + 1],
                              in1=p_S[:, h * 96:(h + 1) * 96], op0=ALU.mult, op1=ALU.add)
                        A("pool", "tensor_copy", [S[d]], [Sb[d]], out=Sb[d][:], in_=S[d][:])
                        if d == 1 and not (last and ti < 2):
                            A("act", "activation", [z], [sg[r]], out=sg[r][:], in_=z[:, 768:1152], func=AF.Silu)
                            for h in range(4):
                                A("act", "activation", [(O, ti)], [junk, (ssq[r], h)], out=junk[:],
                                  in_=O[:, ti, h * 96:(h + 1) * 96], func=AF.Square, accum_out=ssq[r][:, h:h + 1])
                            A("act", "activation", [ssq[r], eps_t], [ssq[r]], out=ssq[r][:], in_=ssq[r][:], func=AF.Sqrt,
                              scale=1.0 / 96, bias=eps_t[:, 0:1])
                            A("dve", "reciprocal", [ssq[r]], [ssq[r]], out=ssq[r][:], in_=ssq[r][:])
                            for h in range(4):
                                A("dve", "scalar_tensor_tensor", [(O, ti), ssq[r], gng], [(yn[r], h)],
                                  out=yn[r][:, h * 96:(h + 1) * 96], in0=O[:, ti, h * 96:(h + 1) * 96],
                                  scalar=ssq[r][:, h:h + 1], in1=gng[:], op0=ALU.mult, op1=ALU.mult)
                            A("pool", "tensor_tensor", [yn[r], sg[r]], [yo[r]], out=yo[r][:], in0=yn[r][:], in1=sg[r][:],
                              op=ALU.mult)
                            DMA([yo[r]], [(YM, ("gla", ti))], out=YM[ti * 128:(ti + 1) * 128, 256:640], in_=yo[r][:])
                    prev = None
                    for it, ti in enumerate(order):
                        g_ = chunk_gen(it, ti)
                        next(g_, None)
                        if prev is not None:
                            for _ in prev:
                                pass
                        prev = g_
                    if prev is not None:
                        for _ in prev:
                            pass

        def zero_pad_rows():
            with Scope(B) as st:
                zr = B.sb(st, "zrow", [4, N_IN], F32)
                A("pool", "memset", [], [zr], ap=zr[:], constant=0.0)
                DMA([zr], [(Z, "pad0")], out=Z[0:4, :], in_=zr[:])
                DMA([zr], [(Z, "pad1")], out=Z[4 + T:8 + T, :], in_=zr[:])

        def conv_taps(zsh, acc, tmp, wk, cm, tt, ntap, width, zcol0, ti):
            half = ntap // 2
            for k in range(ntap):
                sft = k - half
                zs = zsh[(ti * ntap + k) % len(zsh)]
                r0 = 4 + ti * 128 + sft
                DMA([Z], [zs], out=zs[:, 0:width], in_=Z[r0:r0 + 128, zcol0:zcol0 + width])
                dst = acc if k == 0 else tmp
                A("dve", "scalar_tensor_tensor", [zs, cm, wk], [dst], out=dst[:, 0:width], in0=zs[:, 0:width],
                  scalar=cm[:, tt, 3 + sft:4 + sft], in1=wk[:, k, 0:width], op0=ALU.mult, op1=ALU.mult)
                if k > 0:
                    A("pool", "tensor_tensor", [acc, tmp], [acc], out=acc[:, 0:width], in0=acc[:, 0:width],
                      in1=tmp[:, 0:width], op=ALU.add)

        def gdn_prep(layer):
            with Scope(B) as st:
                wk = B.sb(st, "gwk", [128, 7, 1152], F32)
                cm = B.sb(st, "gcm", [128, 3, 7], F32)
                par = B.sb(st, "gpar", [128, 16], F32)
                negA = B.sb(st, "gnegA", [128, 8], F32)
                DMA([gdncw_in], [wk], out=wk[:].rearrange("p k c -> p (k c)"),
                    in_=gdncw_in[layer].partition_broadcast(128))
                DMA([cmask_in], [cm], out=cm[:], in_=cmask_in[:])
                DMA([gdnpar_in], [par], out=par[:], in_=gdnpar_in[layer].partition_broadcast(128))
                A("act", "activation", [par], [negA], out=negA[:], in_=par[:, 0:8], func=AF.Exp)
                A("act", "activation", [negA], [negA], out=negA[:], in_=negA[:], func=AF.Copy, scale=-1.0)
                zsh = B.ring(st, "gzsh", [128, 1152], F32, 14)
                acc = B.ring(st, "gacc", [128, 1152], F32, 2)
                tmp = B.sb(st, "gtmp", [128, 1152], F32)
                u = B.ring(st, "gu", [128, 1152], F32, 2)
                sq = B.sb(st, "gsq", [128, 768], F32)
                ssq = B.ring(st, "gssq", [128, 8], F32, 2)
                ob = B.ring(st, "gob", [128, 1152], BF16, 2)
                zp = B.ring(st, "gzp", [128, 16], F32, 2)
                t8 = B.ring(st, "gt8", [128, 16], F32, 2)
                gp = B.ring(st, "ggp", [128, 24], F32, 2)
                for ti in range(NT):
                    r = ti % 2
                    tt = 0 if ti >= 2 else (1 + ti)
                    conv_taps(zsh, acc[r], tmp, wk, cm, tt, 7, 1152, GDN0, ti)
                    A("act", "activation", [acc[r]], [u[r]], out=u[r][:], in_=acc[r][:], func=AF.Silu)
                    A("pool", "tensor_tensor", [u[r]], [sq], out=sq[:], in0=u[r][:, 0:768], in1=u[r][:, 0:768], op=ALU.mult)
                    A("dve", "tensor_reduce", [sq], [ssq[r]], out=ssq[r][:], in_=sq[:].rearrange("p (h d) -> p h d", d=96),
                      axis=mybir.AxisListType.X, op=ALU.add)
                    A("act", "activation", [ssq[r], eps_t], [ssq[r]], out=ssq[r][:], in_=ssq[r][:], func=AF.Sqrt,
                      bias=eps_t[:, 0:1])
                    A("dve", "reciprocal", [ssq[r]], [ssq[r]], out=ssq[r][:], in_=ssq[r][:])
                    A("act", "activation", [ssq[r]], [ssq[r]], out=ssq[r][:, 0:4], in_=ssq[r][:, 0:4], func=AF.Copy,
                      scale=96.0 ** -0.5)
                    A("dve", "tensor_tensor", [u[r], ssq[r]], [ob[r]],
                      out=ob[r][:, 0:768].rearrange("p (h d) -> p h d", d=96),
                      in0=u[r][:, 0:768].rearrange("p (h d) -> p h d", d=96),
                      in1=ssq[r][:, 0:8].unsqueeze(2).to_broadcast([128, 8, 96]), op=ALU.mult)
                    A("pool", "tensor_copy", [u[r]], [ob[r]], out=ob[r][:, 768:1152], in_=u[r][:, 768:1152])
                    DMA([ob[r]], [(GQKV, ti)], out=GQKV[ti * 128:(ti + 1) * 128, :], in_=ob[r][:])
                    DMA([(Z, ti)], [zp[r]], out=zp[r][:], in_=Z[4 + ti * 128:4 + (ti + 1) * 128, GDN0 + 1536:GDN0 + 1552])
                    A("dve", "tensor_tensor", [zp[r], par], [t8[r]], out=t8[r][:, 0:8], in0=zp[r][:, 0:8], in1=par[:, 8:16],
                      op=ALU.add)
                    A("act", "activation", [t8[r]], [t8[r]], out=t8[r][:, 0:8], in_=t8[r][:, 0:8], func=AF.Exp)
                    A("act", "activation", [t8[r]], [t8[r]], out=t8[r][:, 0:8], in_=t8[r][:, 0:8], func=AF.Ln, bias=1.0)
                    A("dve", "tensor_tensor", [t8[r], negA], [gp[r]], out=gp[r][:, 0:8], in0=t8[r][:, 0:8], in1=negA[:],
                      op=ALU.mult)
                    A("act", "activation", [zp[r]], [t8[r]], out=t8[r][:, 8:16], in_=zp[r][:, 8:16], func=AF.Exp, scale=-1.0)
                    A("act", "activation", [t8[r]], [t8[r]], out=t8[r][:, 8:16], in_=t8[r][:, 8:16], func=AF.Ln, bias=1.0)
                    A("act", "activation", [t8[r]], [gp[r]], out=gp[r][:, 8:16], in_=t8[r][:, 8:16], func=AF.Copy, scale=-1.0)
                    A("act", "activation", [gp[r]], [gp[r]], out=gp[r][:, 16:24], in_=gp[r][:, 8:16], func=AF.Exp)
                    DMA([gp[r]], [(GPAR, ti)], out=GPAR[ti * 128:(ti + 1) * 128, :], in_=gp[r][:])

        def gdn_mixer(layer, last):
            gdn_prep(layer)
            if stop_after == ("GDNPREP", layer):
                return
            with Scope(B) as st:
                tri1 = B.sb(st, "tri1", [128, 4, 128], F32)
                nmk = B.sb(st, "nmk", [128, 4, 128], F32)
                esel = B.sb(st, "esel", [8, 8, 128], F32)
                id4 = B.sb(st, "id4", [128, 512], BF16)
                gng = B.sb(st, "dng", [128, 96], F32)
                DMA([tri1_in], [tri1], out=tri1[:], in_=tri1_in[:])
                DMA([nmask_in], [nmk], out=nmk[:], in_=nmask_in[:])
                DMA([esel_in], [esel], out=esel[:], in_=esel_in[:])
                DMA([gdnng_in], [gng], out=gng[:], in_=gdnng_in[layer:layer + 1, :].partition_broadcast(128))
                for h in range(4):
                    A("pool", "tensor_copy", [ident], [id4], out=id4[:, h * 128:(h + 1) * 128], in_=ident[:])
                O = B.sb(st, "gdnO", [128, NT, 384], F32)
                Sd = [B.sb(st, "gdnS%d" % d, [96, 384], F32) for d in range(2)]
                Sb = [B.sb(st, "gdnSb%d" % d, [96, 384], BF16) for d in range(2)]
                R2 = lambda name, shape, dt: B.ring(st, name, shape, dt, 2)
                qkv = R2("dqkv", [128, 1152], BF16)
                gp = R2("dgp", [128, 24], F32)
                gc = R2("dgc", [128, 8], F32)
                gct = R2("dgct", [128, 8], F32)
                gT = R2("dgT", [8, 128], F32)
                ngT = R2("dngT", [8, 128], F32)
                esc = R2("desc", [128, 8], F32)
                DT = R2("dDT", [128, 512], F32)
                DsB = R2("dDsB", [128, 512], F32)
                egT = R2("degT", [96, 512], F32)
                kT = R2("dkT", [96, 512], BF16)
                nkT = R2("dnkT", [96, 512], BF16)
                qT = R2("dqT", [96, 512], BF16)
                qtT = R2("dqtT", [96, 512], BF16)
                Am = R2("dA", [128, 512], BF16)
                Bm = R2("dB", [128, 512], BF16)
                Ball = R2("dBall", [128, 7, 512], BF16)
                Aall = R2("dAall", [128, 7, 512], BF16)
                P1s = R2("dP1s", [128, 512], BF16)
                P1t = R2("dP1t", [128, 512], BF16)
                Yfin = R2("dYfin", [128, 512], BF16)
                gmf = B.sb(st, "gmf", [128, 7, 128], F32)
                gmk = B.sb(st, "gmk", [128, 7, 512], BF16)
                DMA([gmask_in], [gmf], out=gmf[:], in_=gmask_in[:])
                for h in range(4):
                    A("pool", "tensor_copy", [gmf], [gmk], out=gmk[:, :, h * 128:(h + 1) * 128], in_=gmf[:])
                atT = R2("datT", [128, 512], BF16)
                ST = B.ring(st, "dST", [128, 512], BF16, 3)
                Y = B.ring(st, "dY", [128, 512], BF16, 3)
                nWT = R2("dnWT", [96, 512], BF16)
                rw = R2("drw", [128, 384], BF16)
                ru = R2("dru", [128, 384], BF16)
                kp = R2("dkp", [128, 384], BF16)
                vn = R2("dvn", [128, 384], BF16)
                zgate = R2("dzg", [128, 384], F32)
                sg = R2("dsg", [128, 384], F32)
                ssq = R2("dssq", [128, 4], F32)
                yn = R2("dyn", [128, 384], F32)
                yo = R2("dyo", [128, 384], BF16)
                junk = B.sb(st, "djunk", [128, 96], F32)
                pf = B.ring(st, "dpf", [128, 512], F32, 6, psum=True)
                pb = B.ring(st, "dpb", [128, 1024], BF16, 2, psum=True)
                cnt = {"f": 0, "b": 0}

                def PF():
                    cnt["f"] += 1
                    return pf[cnt["f"] % 6]

                def PB():
                    cnt["b"] += 1
                    return pb[cnt["b"] % 2]

                sidx = 0
                for d in range(2):
                    A("pool", "memset", [], [Sd[d]], ap=Sd[d][:], constant=0.0)
                    A("pool", "memset", [], [Sb[d]], ap=Sb[d][:], constant=0.0)
                    if B.debug and d == 1:
                        dOf = B.dram("dbg_Of", [128, NT, 384], F32, kind="ExternalOutput")
                        DMA([O], [dOf], out=dOf[:], in_=O[:])
                        dSf = B.dram("dbg_Sf", [96, 384], F32, kind="ExternalOutput")
                        DMA([Sd[0]], [dSf], out=dSf[:], in_=Sd[0][:])
                    order = list(range(NT)) if d == 0 else [1, 0] + list(range(NT - 1, 1, -1))
                    gstop = stop_after[1] if (stop_after and stop_after[0] == "GDN") else 99
                    if gstop < 99:
                        order = order[:3]
                    col = 127 if d == 0 else 0
                    def chunk_gen(it, ti, d=d, col=col, gstop=gstop):
                        nonlocal sidx
                        r = it % 2
                        DMA([(GQKV, ti)], [qkv[r]], out=qkv[r][:], in_=GQKV[ti * 128:(ti + 1) * 128, :])
                        DMA([(GPAR, ti)], [gp[r]], out=gp[r][:], in_=GPAR[ti * 128:(ti + 1) * 128, :])
                        g_d = gp[r][:, d * 4:(d + 1) * 4]
                        p = PF()
                        MM([tri1, gp[r]], [p], p[:, 0:4], lhsT=tri1[:, d, :], rhs=g_d, start=True, stop=True)
                        MM([tri1, gp[r]], [p], p[:, 4:8], lhsT=tri1[:, 2 + d, :], rhs=g_d, start=True, stop=True)
                        B.copy("act", p, gc[r], out=gc[r][:], in_=p[:, 0:8])
                        A("pool", "tensor_copy", [gc[r]], [gct[r]], out=gct[r][:, 0:4], in_=gc[r][:, 0:4])
                        A("dve", "tensor_tensor", [gc[r], gp[r]], [gct[r]], out=gct[r][:, 4:8], in0=gc[r][:, 0:4],
                          in1=gp[r][:, 8 + d * 4:12 + d * 4], op=ALU.add)
                        A("act", "activation", [gct[r]], [esc[r]], out=esc[r][:, 0:4], in_=gct[r][:, 4:8], func=AF.Exp)
                        A("act", "activation", [gc[r]], [esc[r]], out=esc[r][:, 4:8], in_=gc[r][:, 4:8], func=AF.Exp)
                        p = PF()
                        TR([gct[r], identf], [p], out=p[0:8, 0:128], in_=gct[r][:], ident=identf[:])
                        B.copy("act", p, gT[r], out=gT[r][:], in_=p[0:8, 0:128])
                        A("act", "activation", [p], [ngT[r]], out=ngT[r][:], in_=p[0:8, 0:128], func=AF.Copy, scale=-1.0)
                        if gstop <= 1:
                            return
                        p1 = PF()
                        p2 = PF()
                        p3 = PF()
                        for h in range(4):
                            hs = slice(h * 128, (h + 1) * 128)
                            MM([esel, gT[r]], [p1], p1[:, hs], lhsT=esel[:, h, :], rhs=gT[r][:], start=True, stop=False)
                            MM([ngT[r], esel], [p1], p1[:, hs], lhsT=ngT[r][:], rhs=esel[:, h, :], start=False, stop=False)
                            MM([identf, nmk], [p1], p1[:, hs], lhsT=identf[:], rhs=nmk[:, d, :], start=False, stop=True)
                            MM([gT[r], esel], [p2], p2[:, hs], lhsT=gT[r][:], rhs=esel[:, 4 + h, :], start=True, stop=False)
                            MM([esel, ngT[r]], [p2], p2[:, hs], lhsT=esel[:, h, :], rhs=ngT[r][:], start=False, stop=False)
                            MM([identf, nmk], [p2], p2[:, hs], lhsT=identf[:], rhs=nmk[:, 2 + d, :], start=False, stop=True)
                            MM([esel, gT[r]], [p3], p3[0:96, hs], lhsT=esel[:, h, 0:96], rhs=gT[r][:], start=True, stop=True)
                        A("act", "activation", [p1], [DT[r]], out=DT[r][:], in_=p1[:], func=AF.Exp)
                        A("act", "activation", [p2], [DsB[r]], out=DsB[r][:], in_=p2[:], func=AF.Exp)
                        A("act", "activation", [p3], [egT[r]], out=egT[r][:], in_=p3[0:96, :], func=AF.Exp)
                        if gstop <= 2:
                            return
                        pq = PB()
                        for j in range(8):
                            TR([qkv[r], ident], [pq], out=pq[0:96, j * 128:(j + 1) * 128], in_=qkv[r][:, j * 96:(j + 1) * 96],
                               ident=ident[:], inc=(j == 7))
                        B.copy("dve", pq, qT[r], out=qT[r][:], in_=pq[0:96, 0:512])
                        A("dve", "tensor_tensor", [pq, egT[r]], [qtT[r]], out=qtT[r][:], in0=pq[0:96, 0:512], in1=egT[r][:],
                          op=ALU.mult)
                        B.copy("dve", pq, kT[r], out=kT[r][:], in_=pq[0:96, 512:1024])
                        if gstop <= 3:
                            return
                        pk = PF()
                        pa = PF()
                        for h in range(4):
                            hs = slice(h * 128, (h + 1) * 128)
                            MM([kT[r]], [pk], pk[:, hs], lhsT=kT[r][:, hs], rhs=kT[r][:, hs], start=True, stop=True)
                            MM([kT[r], qT[r]], [pa], pa[:, hs], lhsT=kT[r][:, hs], rhs=qT[r][:, hs], start=True, stop=True)
                        A("dve", "scalar_tensor_tensor", [pk, DsB[r]], [Am[r]], out=Am[r][:], in0=pk[:], scalar=-1.0,
                          in1=DsB[r][:], op0=ALU.mult, op1=ALU.mult)
                        A("dve", "tensor_tensor", [pa, DT[r]], [atT[r]], out=atT[r][:], in0=pa[:], in1=DT[r][:], op=ALU.mult)
                        if gstop <= 4:
                            return
                        pt = PB()
                        for h in range(4):
                            TR([Am[r], ident], [pt], out=pt[:, h * 128:(h + 1) * 128], in_=Am[r][:, h * 128:(h + 1) * 128],
                               ident=ident[:], inc=(h == 3))
                        B.copy("dve", pt, Bm[r], out=Bm[r][:], in_=pt[:, 0:512])
                        A("dve", "tensor_tensor", [gmk, Bm[r]], [Ball[r]], out=Ball[r][:], in0=gmk[:],
                          in1=Bm[r][:].unsqueeze(1).to_broadcast([128, 7, 512]), op=ALU.mult)
                        A("pool", "tensor_tensor", [gmk, Am[r]], [Aall[r]], out=Aall[r][:], in0=gmk[:],
                          in1=Am[r][:].unsqueeze(1).to_broadcast([128, 7, 512]), op=ALU.mult)
                        y_cur = Y[sidx % 3]
                        yt_cur = ST[sidx % 3]
                        sidx += 1
                        A("pool", "tensor_tensor", [Ball[r], id4], [y_cur], out=y_cur[:], in0=Ball[r][:, 6, :], in1=id4[:], op=ALU.add)
                        A("pool", "tensor_tensor", [Aall[r], id4], [yt_cur], out=yt_cur[:], in0=Aall[r][:, 6, :], in1=id4[:], op=ALU.add)
                        for lvl in range(6):
                            fin = lvl == 5
                            y_new = Yfin[r] if fin else Y[sidx % 3]
                            yt_new = ST[sidx % 3]
                            sidx += 1
                            p1 = PF()
                            for h in range(4):
                                hs = slice(h * 128, (h + 1) * 128)
                                MM([Aall[r], y_cur], [p1], p1[:, hs], lhsT=Aall[r][:, lvl, hs], rhs=y_cur[:, hs], start=True, stop=True)
                            B.copy("act", p1, P1s[r], out=P1s[r][:], in_=p1[:])
                            if not fin:
                                p1t = PF()
                                for h in range(4):
                                    hs = slice(h * 128, (h + 1) * 128)
                                    MM([Ball[r], yt_cur], [p1t], p1t[:, hs], lhsT=Ball[r][:, lvl, hs], rhs=yt_cur[:, hs], start=True, stop=True)
                                B.copy("act", p1t, P1t[r], out=P1t[r][:], in_=p1t[:])
                            p2 = PF()
                            for h in range(4):
                                hs = slice(h * 128, (h + 1) * 128)
                                MM([yt_cur, P1s[r]], [p2], p2[:, hs], lhsT=yt_cur[:, hs], rhs=P1s[r][:, hs], start=True, stop=True)
                            A("dve", "tensor_tensor", [p2, y_cur], [y_new], out=y_new[:], in0=p2[:], in1=y_cur[:], op=ALU.add)
                            if not fin:
                                p2t = PF()
                                for h in range(4):
                                    hs = slice(h * 128, (h + 1) * 128)
                                    MM([y_cur, P1t[r]], [p2t], p2t[:, hs], lhsT=y_cur[:, hs], rhs=P1t[r][:, hs], start=True, stop=True)
                                A("dve", "tensor_tensor", [p2t, yt_cur], [yt_new], out=yt_new[:], in0=p2t[:], in1=yt_cur[:], op=ALU.add)
                            y_cur, yt_cur = y_new, yt_new
                        if B.debug and d == 0 and it == 0:
                            for nm, tl, shp, dt_ in (("dDT", DT[r], [128, 512], F32), ("dDsB", DsB[r], [128, 512], F32),
                                                     ("dAm", Am[r], [128, 512], BF16), ("dY", y_cur, [128, 512], BF16),
                                                     ("datT", atT[r], [128, 512], BF16), ("dqtT", qtT[r], [96, 512], BF16),
                                                     ("dkT", kT[r], [96, 512], BF16), ("degT", egT[r], [96, 512], F32),
                                                     ("dgc", gc[r], [128, 8], F32), ("desc", esc[r], [128, 8], F32)):
                                dd_ = B.dram("dbg_" + nm, shp, dt_, kind="ExternalOutput")
                                DMA([tl], [dd_], out=dd_[:], in_=tl[:])
                        k3 = qkv[r][:, 384:768].rearrange("p (h d) -> p h d", d=96)
                        v3 = qkv[r][:, 768:1152].rearrange("p (h d) -> p h d", d=96)
                        bc = lambda ap: ap.unsqueeze(2).to_broadcast([128, 4, 96])
                        A("dve", "tensor_tensor", [qkv[r], esc[r]], [rw[r]], out=rw[r][:].rearrange("p (h d) -> p h d", d=96),
                          in0=k3, in1=bc(esc[r][:, 0:4]), op=ALU.mult)
                        A("dve", "tensor_tensor", [qkv[r], gp[r]], [ru[r]], out=ru[r][:].rearrange("p (h d) -> p h d", d=96),
                          in0=v3, in1=bc(gp[r][:, 16 + d * 4:20 + d * 4]), op=ALU.mult)
                        A("dve", "tensor_tensor", [qkv[r], esc[r]], [kp[r]], out=kp[r][:].rearrange("p (h d) -> p h d", d=96),
                          in0=k3, in1=bc(esc[r][:, 4:8]), op=ALU.mult)
                        pw = PF()
                        for h in range(4):
                            MM([rw[r], y_cur], [pw], pw[0:96, h * 128:(h + 1) * 128], lhsT=rw[r][:, h * 96:(h + 1) * 96],
                               rhs=y_cur[:, h * 128:(h + 1) * 128], start=True, stop=True)
                        A("act", "activation", [pw], [nWT[r]], out=nWT[r][:], in_=pw[0:96, :], func=AF.Copy, scale=-1.0)
                        if gstop <= 7:
                            return
                        yield
                        pv = PF()
                        for h in range(4):
                            MM([y_cur, ru[r]], [pv], pv[:, h * 96:(h + 1) * 96], lhsT=y_cur[:, h * 128:(h + 1) * 128],
                               rhs=ru[r][:, h * 96:(h + 1) * 96], start=True, stop=False)
                            MM([nWT[r], Sb[d]], [pv], pv[:, h * 96:(h + 1) * 96], lhsT=nWT[r][:, h * 128:(h + 1) * 128],
                               rhs=Sb[d][:, h * 96:(h + 1) * 96], start=False, stop=True)
                        B.copy("act", pv, vn[r], out=vn[r][:], in_=pv[:, 0:384])
                        if B.debug and d == 0 and it == 0:
                            for nm, tl, shp, dt_ in (("dvn", vn[r], [128, 384], BF16), ("dnWT", nWT[r], [96, 512], BF16),
                                                     ("drw", rw[r], [128, 384], BF16), ("dru", ru[r], [128, 384], BF16),
                                                     ("dkp", kp[r], [128, 384], BF16)):
                                dd_ = B.dram("dbg_" + nm, shp, dt_, kind="ExternalOutput")
                                DMA([tl], [dd_], out=dd_[:], in_=tl[:])
                        po = PF()
                        for h in range(4):
                            MM([qtT[r], Sb[d]], [po], po[:, h * 96:(h + 1) * 96], lhsT=qtT[r][:, h * 128:(h + 1) * 128],
                               rhs=Sb[d][:, h * 96:(h + 1) * 96], start=True, stop=False)
                            MM([atT[r], vn[r]], [po], po[:, h * 96:(h + 1) * 96], lhsT=atT[r][:, h * 128:(h + 1) * 128],
                               rhs=vn[r][:, h * 96:(h + 1) * 96], start=False, stop=True)
                        if d == 0:
                            B.copy("act", po, (O, ti), out=O[:, ti, :], in_=po[:, 0:384])
                        else:
                            A("dve", "tensor_tensor", [po, (O, ti)], [(O, ti)], out=O[:, ti, :], in0=po[:, 0:384],
                              in1=O[:, ti, :], op=ALU.add)
                        pS = PF()
                        for h in range(4):
                            MM([kp[r], vn[r]], [pS], pS[0:96, h * 96:(h + 1) * 96], lhsT=kp[r][:, h * 96:(h + 1) * 96],
                               rhs=vn[r][:, h * 96:(h + 1) * 96], start=True, stop=True)
                        for h in range(4):
                            A("dve", "scalar_tensor_tensor", [Sd[d], egT[r], pS], [Sd[d]], out=Sd[d][:, h * 96:(h + 1) * 96],
                              in0=Sd[d][:, h * 96:(h + 1) * 96], scalar=egT[r][:, h * 128 + col:h * 128 + col + 1],
                              in1=pS[0:96, h * 96:(h + 1) * 96], op0=ALU.mult, op1=ALU.add)
                        A("pool", "tensor_copy", [Sd[d]], [Sb[d]], out=Sb[d][:], in_=Sd[d][:])
                        if d == 1 and not (last and ti < 2):
                            DMA([(Z, ti)], [zgate[r]], out=zgate[r][:],
                                in_=Z[4 + ti * 128:4 + (ti + 1) * 128, GDN0 + 1152:GDN0 + 1536])
                            A("act", "activation", [zgate[r]], [sg[r]], out=sg[r][:], in_=zgate[r][:], func=AF.Silu)
                            for h in range(4):
                                A("act", "activation", [(O, ti)], [junk, (ssq[r], h)], out=junk[:],
                                  in_=O[:, ti, h * 96:(h + 1) * 96], func=AF.Square, accum_out=ssq[r][:, h:h + 1])
                            A("act", "activation", [ssq[r], eps_t], [ssq[r]], out=ssq[r][:], in_=ssq[r][:], func=AF.Sqrt,
                              scale=1.0 / 96, bias=eps_t[:, 0:1])
                            A("dve", "reciprocal", [ssq[r]], [ssq[r]], out=ssq[r][:], in_=ssq[r][:])
                            for h in range(4):
                                A("dve", "scalar_tensor_tensor", [(O, ti), ssq[r], gng], [(yn[r], h)],
                                  out=yn[r][:, h * 96:(h + 1) * 96], in0=O[:, ti, h * 96:(h + 1) * 96],
                                  scalar=ssq[r][:, h:h + 1], in1=gng[:], op0=ALU.mult, op1=ALU.mult)
                            A("pool", "tensor_tensor", [yn[r], sg[r]], [yo[r]], out=yo[r][:], in0=yn[r][:], in1=sg[r][:],
                              op=ALU.mult)
                            DMA([yo[r]], [(YM, ("gdn", ti))], out=YM[ti * 128:(ti + 1) * 128, 640:1024], in_=yo[r][:])
                    prev = None
                    for it, ti in enumerate(order):
                        g_ = chunk_gen(it, ti)
                        next(g_, None)
                        if prev is not None:
                            for _ in prev:
                                pass
                        prev = g_
                    if prev is not None:
                        for _ in prev:
                            pass
                if B.debug:
                    dOa = B.dram("dbg_Oa", [128, NT, 384], F32, kind="ExternalOutput")
                    DMA([O], [dOa], out=dOa[:], in_=O[:])


        PI = math.pi

        def hy_cast_tables():
            with Scope(B) as st:
                stg = B.ring(st, "mtstg", [128, 5, 640], F32, 2)
                stb = B.ring(st, "mtstb", [128, 5, 640], BF16, 2)
                for i in range(13):
                    r = i % 2
                    DMA([mt_in], [stg[r]], out=stg[r][:], in_=mt_in[:, i * 5:(i + 1) * 5].rearrange("p f v m -> p f (v m)"))
                    B.copy(("dve", "pool")[r], stg[r], stb[r], out=stb[r][:], in_=stg[r][:])
                    DMA([stb[r]], [(MTB, i)], out=MTB[:, i * 5:(i + 1) * 5].rearrange("p f v m -> p f (v m)"), in_=stb[r][:])

        def hy_conv_pass(layer):
            with Scope(B) as st:
                wk = B.sb(st, "hwk", [128, 3, 768], F32)
                cb = B.sb(st, "hcb", [128, 768], F32)
                cm = B.sb(st, "hcm", [128, 3, 7], F32)
                DMA([hycw_in], [wk], out=wk[:].rearrange("p k c -> p (k c)"), in_=hycw_in[layer].partition_broadcast(128))
                DMA([hycb_in], [cb], out=cb[:], in_=hycb_in[layer].partition_broadcast(128))
                DMA([cmask_in], [cm], out=cm[:], in_=cmask_in[:])
                zsh = B.ring(st, "hzsh", [128, 768], F32, 6)
                acc = B.ring(st, "hacc", [128, 768], F32, 2)
                tmp = B.sb(st, "htmp", [128, 768], F32)
                vb = B.ring(st, "hvb", [128, 256], BF16, 2)
                for ti in range(NT):
                    r = ti % 2
                    tt = 0 if ti >= 2 else (1 + ti)
                    conv_taps(zsh, acc[r], tmp, wk, cm, tt, 3, 768, 0, ti)
                    A("pool", "tensor_tensor", [acc[r], cb], [acc[r]], out=acc[r][:], in0=acc[r][:], in1=cb[:], op=ALU.add)
                    A("pool", "tensor_copy", [acc[r]], [vb[r]], out=vb[r][:], in_=acc[r][:, 0:256])
                    DMA([acc[r]], [(HU, ti)], out=HU[ti * 128:(ti + 1) * 128, :], in_=acc[r][:])
                    DMA([vb[r]], [(HV, ti)], out=HV[ti * 128:(ti + 1) * 128, :], in_=vb[r][:])

        def hy_load_small(st, layer):
            f1f = B.sb(st, "f1f", [128, 2, 65], F32)
            f1b = B.sb(st, "f1b", [128, 2, 65], BF16)
            DMA([f1_in], [f1f], out=f1f[:], in_=f1_in[:])
            A("pool", "tensor_copy", [f1f], [f1b], out=f1b[:], in_=f1f[:])
            return f1b

        def hy_stage1(src_b, src_ap, nt2, f1b):
            with Scope(B) as st:
                Ld = B.sb(st, "Ld", [128, 16384], BF16)
                bt = B.ring(st, "bt", [65, 2, 4096], BF16, 2)
                pp = B.ring(st, "s1p", [65, 512], F32, 4, psum=True)
                DMA([src_b], [Ld], out=Ld[0:nt2, :], in_=src_ap.rearrange("(a b) c -> a (b c)", b=64))
                for g in range(4):
                    b_ = bt[g % 2]
                    for s8 in range(8):
                        sl = slice((g * 8 + s8) * 512, (g * 8 + s8 + 1) * 512)
                        for ri in range(2):
                            p = pp[(s8 * 2 + ri) % 4]
                            MM([f1b, Ld], [p], p[:], lhsT=f1b[0:nt2, ri, :], rhs=Ld[0:nt2, sl], start=True, stop=True)
                            B.copy("act" if ri else "dve", p, (b_, (ri, s8)), out=b_[:, ri, s8 * 512:(s8 + 1) * 512], in_=p[:])
                    for ri in range(2):
                        DMA([b_], [(BD, (ri, g))], out=BD[ri, :, g * 16:(g + 1) * 16, :].rearrange("f t c -> f (t c)"),
                            in_=b_[:, ri, :])

        def hy_stage2(variants, consume):
            with Scope(B) as st:
                R = B.sb(st, "Rall", [128, 65, 256], BF16)
                mab = B.sb(st, "mab", [128, 65, 2, 128], BF16)
                for ri in range(2):
                    DMA([BD], [(R, ri)], out=R[ri * 64:(ri + 1) * 64], in_=BD[ri].rearrange("f t c -> t f c"))
                for j, v in enumerate(variants):
                    DMA([MTB], [(mab, j)], out=mab[:, :, j, :], in_=MTB[:, :, v, :])
                px = B.ring(st, "s2p", [128, 512], F32, 2, psum=True)
                state = consume(st, None, None)
                for f2 in range(65):
                    p = px[f2 % 2]
                    for j in range(2):
                        MM([R, mab], [p], p[:, j * 256:(j + 1) * 256], lhsT=mab[:, f2, j, :], rhs=R[:, f2, :], start=True, stop=True)
                    consume(st, f2, p, state)
                consume(st, 65, None, state)

        def hy_filters(layer, variant):
            with Scope(B) as st:
                w1 = B.sb(st, "fw1", [33, 64], F32)
                w2 = B.sb(st, "fw2", [64, 64], F32)
                w3 = B.sb(st, "fw3", [64, 2, 512], F32)
                fb = B.sb(st, "ffb", [64, 4], F32)
                npi = B.sb(st, "npi", [64, 1], F32)
                DMA([hyw1_in], [w1], out=w1[:], in_=hyw1_in[layer])
                DMA([hyw2_in], [w2], out=w2[:], in_=hyw2_in[layer])
                DMA([hyw3_in], [w3], out=w3[:], in_=hyw3_in[layer])
                DMA([hyfb_in], [fb], out=fb[:], in_=hyfb_in[layer])
                A("pool", "memset", [], [npi], ap=npi[:], constant=PI / 2)
                cs = B.ring(st, "fcs", [64, 512], F32, 2)
                s2 = B.ring(st, "fs2", [64, 512], F32, 2)
                zp = B.ring(st, "fzp", [33, 512], F32, 2)
                arg = B.ring(st, "farg", [64, 512], F32, 2)
                h1 = B.ring(st, "fh1", [64, 512], F32, 2)
                h2 = B.ring(st, "fh2", [64, 512], F32, 2)
                dc = B.ring(st, "fdc", [128, 256], F32, 2)
                hf = B.ring(st, "fhf", [128, 2, 256], BF16, 2)
                pm = B.ring(st, "fpm", [64, 512], F32, 2, psum=True)
                po = B.ring(st, "fpo", [128, 512], F32, 2, psum=True)
                for pb in range(16):
                    r = pb % 2
                    DMA([zpos_in], [zp[r]], out=zp[r][:], in_=zpos_in[variant, :, pb * 512:(pb + 1) * 512])
                    src = zp[r]
                    for li, (w, hh) in enumerate(((w1, h1[r]), (w2, h2[r]))):
                        p = pm[li]
                        MM([w, src], [p], p[:], lhsT=w[:], rhs=src[:], start=True, stop=True)
                        A("dve", "tensor_scalar", [p, fb], [arg[r]], out=arg[r][:], in0=p[:], scalar1=fb[:, li:li + 1],
                          scalar2=fb[:, 2 + li:3 + li], op0=ALU.add, op1=ALU.mult)
                        A("act", "activation", [arg[r]], [hh], out=hh[:], in_=arg[r][:], func=AF.Sin, scale=1.0 / 16)
                        A("act", "activation", [arg[r], npi], [cs[r]], out=cs[r][:], in_=arg[r][:], func=AF.Sin, scale=1.0 / 16,
                          bias=npi[:, 0:1])
                        for _dbl in range(4):
                            A("pool", "tensor_tensor", [hh], [s2[r]], out=s2[r][:], in0=hh[:], in1=hh[:], op=ALU.mult)
                            A("dve", "scalar_tensor_tensor", [hh, cs[r]], [hh], out=hh[:], in0=hh[:], scalar=2.0, in1=cs[r][:],
                              op0=ALU.mult, op1=ALU.mult)
                            A("dve", "tensor_scalar", [s2[r]], [cs[r]], out=cs[r][:], in0=s2[r][:], scalar1=-2.0, scalar2=1.0,
                              op0=ALU.mult, op1=ALU.add)
                        src = hh
                    for j in range(4):
                        tix = pb * 4 + j
                        dirsel = 0 if tix < 32 else 1
                        q = (pb * 4 + j) % 2
                        p = po[q]
                        MM([h2[r], w3], [p], p[:], lhsT=h2[r][:, j * 128:(j + 1) * 128], rhs=w3[:, dirsel, :], start=True, stop=True)
                        DMA([dec_in], [dc[q]], out=dc[q][:], in_=dec_in[variant, tix * 128:(tix + 1) * 128, :])
                        A("dve", "tensor_tensor", [p, dc[q]], [hf[q]], out=hf[q][:],
                          in0=p[:].rearrange("p (o c) -> p o c", o=2), in1=dc[q][:].unsqueeze(1).to_broadcast([128, 2, 256]),
                          op=ALU.mult)
                        for o in range(2):
                            DMA([hf[q]], [(HF, (o, tix))], out=HF[o, tix * 128:(tix + 1) * 128, :], in_=hf[q][:, o, :])
            with Scope(B) as st:
                f1b = hy_load_small(st, layer)
                for o in range(2):
                    hy_stage1(HF, HF[o], 128, f1b)

                    def consume(st2, f2, p, state=None, o=o):
                        if f2 is None:
                            return B.ring(st2, "hcb_", [128, 512], BF16, 3)
                        if f2 == 65:
                            return
                        t = state[f2 % 3]
                        B.copy("act" if f2 % 2 else "dve", p, t, out=t[:], in_=p[:])
                        DMA([t], [(HC, (variant, o, f2))], out=HC[variant, o, f2], in_=t[:])
                    hy_stage2((2, 3), consume)

        def hy_conv(layer, variant, row0, nrows, o, src_b, src_ap, dst_b, dst_ap_fn, gcol):
            nt2 = nrows // 64
            with Scope(B) as st:
                f1b = hy_load_small(st, layer)
                hy_stage1(src_b, src_ap, nt2, f1b)

            def consume(st2, f2, p, state=None):
                if f2 is None:
                    me = B.sb(st2, "me", [128, 65, 128], BF16)
                    DMA([MTB], [me], out=me[:], in_=MTB[:, :, 4, :])
                    return {"me": me, "hc": B.ring(st2, "hcl", [128, 512], BF16, 3),
                            "tmp": B.ring(st2, "s2tmp", [128, 512], BF16, 2),
                            "call": B.sb(st2, "Call", [128, 65, 256], BF16),
                            "pc": B.ring(st2, "s2pc", [128, 256], F32, 2, psum=True)}
                if f2 == 65:
                    call = state["call"]
                    for ri in range(2):
                        DMA([call], [(CD, ri)], out=CD[ri].rearrange("f t c -> t f c"), in_=call[ri * 64:(ri + 1) * 64])
                    return
                hc = state["hc"][f2 % 3]
                tmp = state["tmp"][f2 % 2]
                pc = state["pc"][f2 % 2]
                DMA([HC], [hc], out=hc[:], in_=HC[variant, o, f2])
                A("dve", "tensor_tensor", [p, hc], [tmp], out=tmp[:], in0=p[:], in1=hc[:], op=ALU.mult)
                MM([state["me"], tmp], [pc], pc[:], lhsT=state["me"][:, f2, :], rhs=tmp[:, 0:256], start=True, stop=False)
                MM([state["me"], tmp], [pc], pc[:], lhsT=state["me"][:, f2, :], rhs=tmp[:, 256:512], start=False, stop=True)
                B.copy("act", pc, (state["call"], f2), out=state["call"][:, f2, :], in_=pc[:])
            hy_stage2((0, 1), consume)
            with Scope(B) as st:
                gf = B.sb(st, "gfin", [65, 2, 64], F32)
                gb = B.sb(st, "gbin", [65, 2, 64], BF16)
                dd = B.sb(st, "ddb", [128, 512], F32)
                DMA([g_in], [gf], out=gf[:], in_=g_in[:])
                A("pool", "tensor_copy", [gf], [gb], out=gb[:], in_=gf[:])
                DMA([hyd2_in], [dd], out=dd[:], in_=hyd2_in[layer, o].partition_broadcast(128))
                Cr = B.sb(st, "Cr", [65, 16384], BF16)
                Ci = B.sb(st, "Ci", [65, 16384], BF16)
                Ld = B.sb(st, "Ld2", [128, 16384], BF16)
                DMA([CD], [Cr], out=Cr[:], in_=CD[0].rearrange("f t c -> f (t c)"))
                DMA([CD], [Ci], out=Ci[:], in_=CD[1].rearrange("f t c -> f (t c)"))
                DMA([src_b], [Ld], out=Ld[0:nt2, :], in_=src_ap.rearrange("(a b) c -> a (b c)", b=64))
                gt = B.ring(st, "gt", [128, 4096], F32, 2)
                t1_ = B.ring(st, "hy_t1", [128, 512], F32, 2)
                t2_ = B.ring(st, "hy_t2", [128, 512], F32, 2)
                ot = B.ring(st, "hy_ot", [128, 4096], BF16, 2)
                py = B.ring(st, "s3p", [128, 512], F32, 3, psum=True)
                HUv = HU[row0:row0 + nrows, gcol:gcol + 256].rearrange("(a b) c -> a b c", b=64)
                for g in range(4):
                    g_ = gt[g % 2]
                    o_ = ot[g % 2]
                    DMA([HU], [g_], out=g_[0:nt2, :].rearrange("a (b c) -> a b c", c=256), in_=HUv[:, g * 16:(g + 1) * 16, :])
                    for s8 in range(8):
                        s = g * 8 + s8
                        sl = slice(s * 512, (s + 1) * 512)
                        p = py[s % 3]
                        MM([gb, Cr], [p], p[0:nt2, :], lhsT=gb[:, 0, 0:nt2], rhs=Cr[:, sl], start=True, stop=False)
                        MM([gb, Ci], [p], p[0:nt2, :], lhsT=gb[:, 1, 0:nt2], rhs=Ci[:, sl], start=False, stop=True)
                        a_ = t1_[s % 2]
                        b_ = t2_[s % 2]
                        A("pool", "tensor_tensor", [Ld, dd], [a_], out=a_[0:nt2, :], in0=Ld[0:nt2, sl], in1=dd[0:nt2, :], op=ALU.mult)
                        A("dve", "tensor_tensor", [p, a_], [b_], out=b_[0:nt2, :], in0=p[0:nt2, :], in1=a_[0:nt2, :], op=ALU.add)
                        A("pool", "tensor_tensor", [b_, g_], [(o_, s8)], out=o_[0:nt2, s8 * 512:(s8 + 1) * 512], in0=b_[0:nt2, :],
                          in1=g_[0:nt2, s8 * 512:(s8 + 1) * 512], op=ALU.mult)
                    DMA([o_], [dst_b], out=dst_ap_fn(g), in_=o_[0:nt2, :].rearrange("a (b c) -> a b c", c=256))

        def hyena_mixer(layer, last):
            hy_conv_pass(layer)
            seqs = [(0, 256, SEQ)]
            if not last:
                seqs.append((1, 0, CTX))
            for variant, row0, nrows in seqs:
                hy_filters(layer, variant)
                y1v = HY1[row0:row0 + nrows, :].rearrange("(a b) c -> a b c", b=64)
                ymv = YM[row0:row0 + nrows, 0:256].rearrange("(a b) c -> a b c", b=64)
                hy_conv(layer, variant, row0, nrows, 0, HV, HV[row0:row0 + nrows, :], HY1,
                        lambda g, y1v=y1v: y1v[:, g * 16:(g + 1) * 16, :], 256)
                hy_conv(layer, variant, row0, nrows, 1, HY1, HY1[row0:row0 + nrows, :], YM,
                        lambda g, ymv=ymv: ymv[:, g * 16:(g + 1) * 16, :], 512)

        zero_pad_rows()
        hy_cast_tables()
        for layer in range(DEPTH):
            last = layer == DEPTH - 1
            only_gdn = bool(stop_after and (len(stop_after) > 2 or stop_after[0] == "HY"))
            with Scope(B) as st:
              if not only_gdn:
                cc = B.sb(st, "cc", [128, 8, 2], F32)
                scc = B.sb(st, "scc", [128, 8, 2], F32)
                bm = B.sb(st, "bm", [2, 6 * D], F32)
                modrow = B.sb(st, "modrow", [2, 6 * D], F32)
                wst = B.ring(st, "wmst", [128, 8, 512], F32, 2)
                pmod = B.ring(st, "pmod", [2, 512], F32, 2, psum=True)
                DMA([cc_in], [cc], out=cc[:], in_=cc_in[:])
                DMA([bmod_in], [bm], out=bm[:], in_=bmod_in[layer:layer + 1, :].partition_broadcast(2))
                A("act", "activation", [cc], [scc], out=scc[:], in_=cc[:], func=AF.Silu)
                for s in range(12):
                    w = wst[s % 2]
                    pm = pmod[s % 2]
                    DMA([wmod_in], [w], out=w[:], in_=wmod_in[layer, :, :, s * 512:(s + 1) * 512])
                    for k in range(8):
                        MM([scc, w], [pm], pm[:], lhsT=scc[:, k, :], rhs=w[:, k, :], start=(k == 0), stop=(k == 7))
                    A("dve", "tensor_tensor", [pm, bm], [(modrow, s)], out=modrow[:, s * 512:(s + 1) * 512],
                      in0=pm[:], in1=bm[:, s * 512:(s + 1) * 512], op=ALU.add)
                DMA([modrow], [(MOD, layer)], out=MOD[layer], in_=modrow[:])

            def mod_tiles(st, which, segs, norm_g=None):
                res = {}
                for name, seg in segs:
                    t = load_bcast(st, name, (MOD, layer), MOD[layer, which:which + 1, seg * D:(seg + 1) * D])
                    res[name] = t
                return res

            with Scope(B) as st:
              if not only_gdn:
                winb = load_weight_bf16(st, "winb", win_in, win_in[layer], 8, N_IN)
                g1n = load_bcast(st, "g1n", n1g_in, n1g_in[layer:layer + 1, :])
                sh1 = B.sb(st, "sh1", [128, D], F32)
                gs1 = B.sb(st, "gs1", [128, D], F32)
                xr = B.ring(st, "xr", [128, D], F32, 2)
                junk = B.sb(st, "junk", [128, D], BF16)
                rstd = B.ring(st, "rstd", [128, 1], F32, 2)
                tmp = B.sb(st, "tmp", [128, D], F32)
                hb = B.ring(st, "hb", [128, D], BF16, 2)
                hT = B.ring(st, "hT", [128, 8, 128], BF16, 2)
                zt = B.ring(st, "zt", [128, N_IN], F32, 2)
                tp = B.ring(st, "tp", [128, D], BF16, 2, psum=True)
                pz = B.ring(st, "pz", [128, 512], F32, 4, psum=True)
                for ti in range(NT):
                    if ti == 0 or ti == 2:
                        which = 1 if ti == 0 else 0
                        DMA([(MOD, layer)], [sh1], out=sh1[:],
                            in_=MOD[layer, which:which + 1, 0:D].partition_broadcast(128))
                        DMA([(MOD, layer)], [gs1], out=gs1[:],
                            in_=MOD[layer, which:which + 1, D:2 * D].partition_broadcast(128))
                        A("dve", "scalar_tensor_tensor", [gs1, g1n], [gs1], out=gs1[:], in0=gs1[:], scalar=1.0,
                          in1=g1n[:], op0=ALU.add, op1=ALU.mult)
                    r = ti % 2
                    xb_, xap = x_src(layer, ti)
                    DMA([xb_], [xr[r]], out=xr[r][:], in_=xap)
                    rms_rstd(xr[r], junk, rstd[r])
                    A("dve", "scalar_tensor_tensor", [xr[r], rstd[r], gs1], [tmp], out=tmp[:], in0=xr[r][:],
                      scalar=rstd[r][:, 0:1], in1=gs1[:], op0=ALU.mult, op1=ALU.mult)
                    A("pool", "tensor_tensor", [tmp, sh1], [hb[r]], out=hb[r][:], in0=tmp[:], in1=sh1[:], op=ALU.add)
                    transpose_tile(hb[r], lambda k, r=r: hb[r][:, k * 128:(k + 1) * 128], tp[r], hT[r], hT[r][:])
                    for cg in range(7):
                        c0 = cg * 512
                        c1 = min(N_IN, c0 + 512)
                        p = pz[(ti * 7 + cg) % 4]
                        for k in range(8):
                            MM([hT[r], winb], [p], p[:, 0:c1 - c0], lhsT=hT[r][:, k, :], rhs=winb[:, k, c0:c1],
                               start=(k == 0), stop=(k == 7))
                        B.copy(B.evac_eng(), p, (zt[r], cg), out=zt[r][:, c0:c1], in_=p[:, 0:c1 - c0])
                    DMA([zt[r]], [(Z, ti)], out=Z[4 + ti * 128:4 + (ti + 1) * 128, :], in_=zt[r][:])

            if stop_after == ("P1", layer):
                break

            if stop_after and stop_after[0] == "HY":
                hyena_mixer(layer, last)
                break
            if not only_gdn:
                hyena_mixer(layer, last)
                gla_mixer(layer, last)
            gdn_mixer(layer, last)
            if stop_after and (stop_after in (("MIX", layer), ("GDNPREP", layer)) or stop_after[0] == "GDN"):
                break

            tiles5 = range(NT) if not last else range(2, NT)
            with Scope(B) as st:
                woutb = load_weight_bf16(st, "woutb", wout_in, wout_in[layer], 8, D)
                g1t = B.sb(st, "g1t", [128, D], F32)
                xr = B.ring(st, "xr5", [128, D], F32, 2)
                ym = B.ring(st, "ym", [128, D], BF16, 2)
                mT = B.ring(st, "mT", [128, 8, 128], BF16, 2)
                tmp = B.ring(st, "tmp5", [128, 512], F32, 2)
                tp = B.ring(st, "tp5", [128, D], BF16, 2, psum=True)
                py = B.ring(st, "py", [128, 512], F32, 4, psum=True)
                for ti in tiles5:
                    if ti == 0 or ti == 2:
                        which = 1 if ti == 0 else 0
                        DMA([(MOD, layer)], [g1t], out=g1t[:],
                            in_=MOD[layer, which:which + 1, 2 * D:3 * D].partition_broadcast(128))
                    r = ti % 2
                    xb_, xap = x_src(layer, ti)
                    DMA([xb_], [xr[r]], out=xr[r][:], in_=xap)
                    DMA([YM], [ym[r]], out=ym[r][:], in_=YM[ti * 128:(ti + 1) * 128, :])
                    transpose_tile(ym[r], lambda k, r=r: ym[r][:, k * 128:(k + 1) * 128], tp[r], mT[r], mT[r][:])
                    for cg in range(2):
                        p = py[(ti * 2 + cg) % 4]
                        for k in range(8):
                            MM([mT[r], woutb], [p], p[:], lhsT=mT[r][:, k, :], rhs=woutb[:, k, cg * 512:(cg + 1) * 512],
                               start=(k == 0), stop=(k == 7))
                        tt = tmp[cg]
                        A("dve", "tensor_tensor", [p, g1t], [tt], out=tt[:], in0=p[:], in1=g1t[:, cg * 512:(cg + 1) * 512],
                          op=ALU.mult)
                        A("pool", "tensor_tensor", [tt, xr[r]], [xr[r]], out=xr[r][:, cg * 512:(cg + 1) * 512],
                          in0=xr[r][:, cg * 512:(cg + 1) * 512], in1=tt[:], op=ALU.add)
                    DMA([xr[r]], [(XS, ti)], out=XS[ti * 128:(ti + 1) * 128, :], in_=xr[r][:])

            with Scope(B) as st:
                w1b = load_weight_bf16(st, "w1b", w1_in, w1_in[layer], 8, D_FF)
                w2b = load_weight_bf16(st, "w2b", w2_in, w2_in[layer], 32, D)
                g2n = load_bcast(st, "g2n", n2g_in, n2g_in[layer:layer + 1, :])
                sh2 = B.sb(st, "sh2", [128, D], F32)
                gs2 = B.sb(st, "gs2", [128, D], F32)
                g2t = B.sb(st, "g2t", [128, D], F32)
                xr = B.ring(st, "xr6", [128, D], F32, 4)
                junk = B.sb(st, "junk6", [128, D], BF16)
                rstd = B.ring(st, "rstd6", [128, 1], F32, 2)
                tmp = B.sb(st, "tmp6", [128, D], F32)
                hb = B.ring(st, "hb6", [128, D], BF16, 2)
                hT2 = B.ring(st, "hT2", [128, 8, 256], BF16, 2)
                uT = B.sb(st, "uT", [128, 32, 256], BF16)
                rl = B.ring(st, "rl", [128, 256], BF16, 2)
                tt2 = B.ring(st, "tt2", [128, 512], F32, 2)
                tp = B.ring(st, "tp6", [128, D], BF16, 2, psum=True)
                pu = B.ring(st, "pu", [128, 256], F32, 3, psum=True)
                py = B.ring(st, "py6", [128, 512], F32, 2, psum=True)
                groups = [(0, 1)] if not last else []
                groups += [(2 + 2 * g, 3 + 2 * g) for g in range(16)]
                for gi, grp in enumerate(groups):
                    if grp[0] == 0 or grp[0] == 2:
                        which = 1 if grp[0] == 0 else 0
                        for tl, seg in ((sh2, 3), (gs2, 4), (g2t, 5)):
                            DMA([(MOD, layer)], [tl], out=tl[:],
                                in_=MOD[layer, which:which + 1, seg * D:(seg + 1) * D].partition_broadcast(128))
                        A("dve", "scalar_tensor_tensor", [gs2, g2n], [gs2], out=gs2[:], in0=gs2[:], scalar=1.0,
                          in1=g2n[:], op0=ALU.add, op1=ALU.mult)
                    hT = hT2[gi % 2]
                    xs = []
                    for j, ti in enumerate(grp):
                        xt = xr[(gi % 2) * 2 + j]
                        xs.append(xt)
                        DMA([(XS, ti)], [xt], out=xt[:], in_=XS[ti * 128:(ti + 1) * 128, :])
                        rs = rstd[j]
                        rms_rstd(xt, junk, rs)
                        A("dve", "scalar_tensor_tensor", [xt, rs, gs2], [tmp], out=tmp[:], in0=xt[:],
                          scalar=rs[:, 0:1], in1=gs2[:], op0=ALU.mult, op1=ALU.mult)
                        A("pool", "tensor_tensor", [tmp, sh2], [hb[j]], out=hb[j][:], in0=tmp[:], in1=sh2[:], op=ALU.add)
                        transpose_tile(hb[j], lambda k, j=j: hb[j][:, k * 128:(k + 1) * 128], tp[j], (hT, j),
                                       hT[:, :, j * 128:(j + 1) * 128])
                    for m in range(32):
                        p = pu[m % 3]
                        for k in range(8):
                            MM([hT, w1b], [p], p[:], lhsT=w1b[:, k, m * 128:(m + 1) * 128], rhs=hT[:, k, :],
                               start=(k == 0), stop=(k == 7))
                        rr_ = rl[m % 2]
                        A("act", "activation", [p], [rr_], out=rr_[:], in_=p[:], func=AF.Relu)
                        A("pool" if m % 2 else "dve", "tensor_tensor", [rr_], [(uT, m)], out=uT[:, m, :], in0=rr_[:],
                          in1=rr_[:], op=ALU.mult)
                    for j, ti in enumerate(grp):
                        xt = xs[j]
                        for cg in range(2):
                            p = py[cg]
                            for m in range(32):
                                MM([uT, w2b], [p], p[:], lhsT=uT[:, m, j * 128:(j + 1) * 128],
                                   rhs=w2b[:, m, cg * 512:(cg + 1) * 512], start=(m == 0), stop=(m == 31))
                            tt = tt2[cg]
                            A("dve", "tensor_tensor", [p, g2t], [tt], out=tt[:], in0=p[:],
                              in1=g2t[:, cg * 512:(cg + 1) * 512], op=ALU.mult)
                            A("pool", "tensor_tensor", [tt, xt], [xt], out=xt[:, cg * 512:(cg + 1) * 512],
                              in0=xt[:, cg * 512:(cg + 1) * 512], in1=tt[:], op=ALU.add)
                        DMA([xt], [(XS, ti)], out=XS[ti * 128:(ti + 1) * 128, :], in_=xt[:])

        with Scope(B) as st:
            fg = load_bcast(st, "fg", fng_in, fng_in[0:1, :])
            xr = B.ring(st, "xrf", [128, D], F32, 2)
            orr = B.ring(st, "orr", [128, D], F32, 2)
            junk = B.sb(st, "junkf", [128, D], BF16)
            rstd = B.ring(st, "rstdf", [128, 1], F32, 2)
            for ti in range(2, NT):
                r = ti % 2
                DMA([(XS, ti)], [xr[r]], out=xr[r][:], in_=XS[ti * 128:(ti + 1) * 128, :])
                rms_rstd(xr[r], junk, rstd[r])
                A("dve", "scalar_tensor_tensor", [xr[r], rstd[r], fg], [orr[r]], out=orr[r][:], in0=xr[r][:],
                  scalar=rstd[r][:, 0:1], in1=fg[:], op0=ALU.mult, op1=ALU.mult)
                DMA([orr[r]], [(out_d, ti)], out=out_d[(ti - 2) * 128:(ti - 1) * 128, :], in_=orr[r][:])

        P._emit_waits("sp", [t for t in P.dma_last_tok if t is not None])
        P.emit(top)
    return nc


def mix_stub(B, layer, YM):
    with Scope(B) as st:
        z = B.sb(st, "zero", [128, D], BF16)
        B.A("pool", "memset", [], [z], ap=z[:], constant=0.0)
        for ti in range(NT):
            B.DMA([z], [(YM, ("hy", ti))], out=YM[ti * 128:(ti + 1) * 128, 0:256], in_=z[:, 0:256])


def _kchunk(w, kch):
    d, r, n = w.shape
    return np.ascontiguousarray(w.reshape(d, kch, 128, n).transpose(0, 2, 1, 3))


def _gdn_consts():
    ii = np.arange(128)
    p, q = ii[:, None], ii[None, :]
    one = lambda c: c.astype(np.float32)
    tri1 = np.stack([one(p <= q), one(p >= q), one(p > q), one(p < q)], axis=1)
    neg = lambda c: np.where(c, 0.0, -30000.0).astype(np.float32)
    nmask = np.stack([neg(p <= q), neg(p >= q), neg(q < p), neg(q > p)], axis=1)
    esel = np.zeros((8, 8, 128), np.float32)
    for k in range(8):
        esel[k, k, :] = 1.0
    cm = np.zeros((128, 3, 7), np.float32)
    for s in range(-3, 4):
        cm[:, 0, s + 3] = ((ii % 64 + s >= 0) & (ii % 64 + s < 64))
        cm[:, 1, s + 3] = (ii + s >= 0)
        cm[:, 2, s + 3] = (128 + ii + s < 256)
    gm = np.zeros((128, 7, 128), np.float32)
    for lv in range(6):
        sz = 2 ** (lv + 1)
        gm[:, lv, :] = ((p // (2 * sz) == q // (2 * sz)) & (p // sz != q // sz))
    gm[:, 6, :] = (p // 2 == q // 2)
    return {"tri1": np.ascontiguousarray(tri1), "nmask": np.ascontiguousarray(nmask), "esel": esel, "cmask": cm, "gmask": gm}


def _hy_consts():
    N = 8192
    t2 = np.arange(128)[:, None]
    f2 = np.arange(65)[None, :]
    ang = 2 * np.pi * t2 * f2 / 128.0
    F1 = np.stack([np.cos(ang), -np.sin(ang)], axis=1)
    t1 = np.arange(64)[:, None]
    f1 = np.arange(64)[None, :]
    MT = np.zeros((65, 128, 5, 128))
    blk = lambda a, b, c, d: np.block([[a, b], [c, d]])
    for k in range(65):
        ph = -2 * np.pi * (t1 * f1 / 64.0 + t1 * k / 8192.0)
        Mr, Mi = np.cos(ph), np.sin(ph)
        MT[k, :, 0] = blk(Mr, Mi, -Mi, Mr)
        MT[k, :, 1] = blk(Mi, Mr, Mr, -Mi)
        MT[k, :, 2] = blk(Mr, Mr, -Mi, -Mi)
        MT[k, :, 3] = blk(-Mi, Mi, -Mr, Mr)
        MT[k, :, 4] = blk(Mr.T, -Mi.T, Mi.T, Mr.T)
    a = np.full(65, 2.0)
    a[0] = 1.0
    a[64] = 1.0
    ang2 = 2 * np.pi * np.arange(64)[None, :] * np.arange(65)[:, None] / 128.0
    G = np.stack([a[:, None] * np.cos(ang2) / N, -a[:, None] * np.sin(ang2) / N], axis=1)
    zpos = np.zeros((2, 33, N), np.float32)
    dec = np.zeros((2, N, 256), np.float32)
    deltas = np.abs(np.linspace(math.log(1e-2) / 1.5, math.log(1e-2) / 0.3, 256, dtype=np.float32))
    fq = np.linspace(1e-4, 15, 16, dtype=np.float32)
    for vi, L in enumerate((4096, 256)):
        t = np.linspace(0.0, 1.0, L, dtype=np.float32)
        w = (2 * math.pi * np.arange(L, dtype=np.float32) / L).astype(np.float32)
        angp = w[:, None] * fq[None, :]
        z = np.concatenate([t[:, None], np.cos(angp), -np.sin(angp)], axis=-1).astype(np.float32)
        dw = np.exp(-t[:, None] * deltas[None, :]).astype(np.float32)
        zpos[vi, :, :L] = z.T
        dec[vi, :L] = dw
        pidx = np.arange(1, L)
        zpos[vi][:, N - pidx] = z[pidx].T
        dec[vi][N - pidx] = dw[pidx]
    return {"hy_F1": F1.astype(np.float32), "hy_MT": np.ascontiguousarray(MT.transpose(1, 0, 2, 3)).astype(np.float32),
            "hy_G": G.astype(np.float32), "hy_zpos": zpos, "hy_dec": dec}


_NC_CACHE = {}


def kernel(x, c, ctx, c_ctx, norm1_g, norm2_g, w_mod, b_mod, w_in, w_out, hy_conv_w, hy_conv_b,
           hy_f_w1, hy_f_b1, hy_f_w2, hy_f_b2, hy_f_w3, hy_sin_freq, hy_d, gla_w_a2, gla_b_a, gla_norm_g,
           gdn_conv_w, gdn_a_log, gdn_dt_bias, gdn_norm_g, w_mlp1, w_mlp2, final_norm_g):
    f = lambda a: np.ascontiguousarray(np.asarray(a, dtype=np.float32))
    x, c, ctx, c_ctx = f(x), f(c), f(ctx), f(c_ctx)
    shared = {
        "norm1_g": f(norm1_g), "norm2_g": f(norm2_g),
        "w_mod": _kchunk(f(w_mod), 8), "b_mod": f(b_mod),
        "w_in": _kchunk(f(w_in), 8), "w_out": _kchunk(f(w_out), 8),
        "w_mlp1": _kchunk(f(w_mlp1), 8), "w_mlp2": _kchunk(f(w_mlp2), 32),
        "final_norm_g": f(final_norm_g).reshape(1, D),
    }
    ii = np.arange(128)
    jle = (ii[:, None] <= ii[None, :]).astype(np.float32)
    jge = (ii[:, None] >= ii[None, :]).astype(np.float32)
    mgt = (ii[:, None] > ii[None, :]).astype(np.float32)
    mlt = (ii[:, None] < ii[None, :]).astype(np.float32)
    shared["tri"] = np.ascontiguousarray(np.stack([jle, jge, mgt, mlt], axis=1) * np.float32(-1.0 / 16.0))
    shared["mask4"] = np.ascontiguousarray(np.stack([np.tile(jle, (1, 4)), np.tile(jge, (1, 4))], axis=1))
    wa = np.zeros((DEPTH, 33, 384), np.float32)
    w_a2, b_a = f(gla_w_a2), f(gla_b_a)
    wa[:, 0:16, 0:192] = w_a2[:, 0]
    wa[:, 16:32, 192:384] = w_a2[:, 1]
    wa[:, 32, 0:192] = b_a[:, 0]
    wa[:, 32, 192:384] = b_a[:, 1]
    shared["gla_wa"] = wa
    shared["gla_ng"] = f(gla_norm_g)
    shared.update(_gdn_consts())
    shared["gdn_cw"] = f(gdn_conv_w).reshape(DEPTH, 1, 7 * 1152)
    shared["gdn_par"] = np.concatenate([f(gdn_a_log).reshape(DEPTH, 8), f(gdn_dt_bias).reshape(DEPTH, 8)],
                                       axis=1).reshape(DEPTH, 1, 16)
    shared["gdn_ng"] = f(gdn_norm_g)
    shared.update(_hy_consts())
    shared["hy_cw"] = f(hy_conv_w).reshape(DEPTH, 1, 3 * 768)
    shared["hy_cb"] = f(hy_conv_b).reshape(DEPTH, 1, 768)
    shared["hy_w1"] = f(hy_f_w1)
    shared["hy_w2"] = f(hy_f_w2)
    shared["hy_w3r"] = np.ascontiguousarray(f(hy_f_w3).reshape(DEPTH, 64, 2, 2, 256).transpose(0, 1, 3, 2, 4)).reshape(DEPTH, 64, 2, 512)
    sf = f(hy_sin_freq)
    shared["hy_fb"] = np.ascontiguousarray(np.stack([f(hy_f_b1), f(hy_f_b2), sf[:, 0], sf[:, 1]], axis=-1))
    shared["hy_d2"] = np.ascontiguousarray(np.tile(f(hy_d), (1, 1, 2)).reshape(DEPTH, 2, 1, 512))
    in_maps = []
    for b in range(8):
        cc = np.stack([c[b], c_ctx], axis=-1).reshape(8, 128, 2).transpose(1, 0, 2)
        m = dict(shared)
        m["x"] = x[b]
        m["ctx"] = ctx[b]
        m["cc"] = np.ascontiguousarray(cc)
        in_maps.append(m)
    if "nc" not in _NC_CACHE:
        _NC_CACHE["nc"] = build_program()
    nc = _NC_CACHE["nc"]
    res = run_bass_kernel_spmd(nc, in_maps, core_ids=list(range(8)))
    return np.stack([np.asarray(r["out"], dtype=np.float32) for r in res.results], axis=0)
```
